# Optimizing a Trainium2 kernel written in Bass

```python
import jax, jax.numpy as jnp
from jax import lax
import numpy as np

D_MODEL = 1024
BATCH = 2
SEQ = 8192
DEPTH = 4

GLA_HEADS = 4
GLA_DK = 128
GLA_DV = 128
GLA_LOWRANK = 16
GLA_GATE_NORM = 16.0
GLA_CHUNK = 64
MOBA_HEADS = 8
MOBA_HD = 64
MOBA_BLOCK = 256
MOBA_TOPK = 3
MOBA_QCHUNK = 64
ROPE_THETA = 10000.0
MLSTM_HEADS = 4
MLSTM_DQK = 64
MLSTM_DV = 128
MLSTM_CHUNK = 64
MLSTM_CONV = 4
D_FF = 4 * D_MODEL
N_BRANCH = 3
EPS = 1e-6

GLA_K = GLA_HEADS * GLA_DK
GLA_V = GLA_HEADS * GLA_DV
MOBA_W = MOBA_HEADS * MOBA_HD
MLSTM_QK = MLSTM_HEADS * MLSTM_DQK
MLSTM_V = MLSTM_HEADS * MLSTM_DV
IN_SPLITS = (GLA_K, GLA_K, GLA_V, GLA_V, GLA_LOWRANK,
             MOBA_W, MOBA_W, MOBA_W,
             MLSTM_QK, MLSTM_QK, MLSTM_V, MLSTM_V, MLSTM_HEADS, MLSTM_HEADS,
             N_BRANCH * D_MODEL)
D_IN = sum(IN_SPLITS)

kernel_name = "hybrid_gla_moba_mlstm_block"


def rms_norm(x, g):
    xf = x.astype(jnp.float32)
    y = xf * lax.rsqrt(jnp.mean(xf * xf, axis=-1, keepdims=True) + EPS)
    return (y * g.astype(jnp.float32)).astype(x.dtype)


def split_heads(x, h):
    b, t, _ = x.shape
    return x.reshape(b, t, h, -1).transpose(0, 2, 1, 3)


def merge_heads(x):
    b, h, t, d = x.shape
    return x.transpose(0, 2, 1, 3).reshape(b, t, h * d)


def rope_tables(seq, dim):
    inv = 1.0 / (ROPE_THETA ** (jnp.arange(0, dim, 2, dtype=jnp.float32) / dim))
    ang = jnp.arange(seq, dtype=jnp.float32)[:, None] * inv[None, :]
    return jnp.cos(ang), jnp.sin(ang)


def apply_rope(x, cos, sin):
    x1, x2 = jnp.split(x, 2, axis=-1)
    return jnp.concatenate([x1 * cos - x2 * sin, x2 * cos + x1 * sin], axis=-1)


def causal_dwconv(x, w):
    wd, c = w.shape
    return lax.conv_general_dilated(x, w[:, None, :], window_strides=(1,), padding=((wd - 1, 0),),
                                    dimension_numbers=('NWC', 'WIO', 'NWC'), feature_group_count=c)


def gla_mixer(q, k, v, log_a):
    b, h, t, dk = q.shape
    dv = v.shape[-1]
    L = GLA_CHUNK
    n = t // L
    qc = (q * dk ** -0.5).reshape(b, h, n, L, dk)
    kc = k.reshape(b, h, n, L, dk)
    vc = v.reshape(b, h, n, L, dv)
    bcum = jnp.cumsum(log_a.reshape(b, h, n, L, dk), axis=3)
    b_last = bcum[:, :, :, -1:, :]
    q_dec = qc * jnp.exp(bcum)
    k_inv = kc * jnp.exp(-bcum)
    k_end = kc * jnp.exp(b_last - bcum)
    causal = jnp.tril(jnp.ones((L, L), dtype=bool))
    attn = jnp.where(causal, jnp.einsum('bhnid,bhnjd->bhnij', q_dec, k_inv), 0.0)
    o_intra = jnp.einsum('bhnij,bhnjv->bhniv', attn, vc)
    chunk_kv = jnp.einsum('bhnld,bhnlv->bhndv', k_end, vc)
    decay = jnp.exp(b_last[:, :, :, 0, :])

    def step(S, inp):
        dec, kv = inp
        return dec[..., None] * S + kv, S

    S0 = jnp.zeros((b, h, dk, dv), q.dtype)
    _, S_prev = lax.scan(step, S0, (jnp.moveaxis(decay, 2, 0), jnp.moveaxis(chunk_kv, 2, 0)))
    S_prev = jnp.moveaxis(S_prev, 0, 2)
    o_inter = jnp.einsum('bhnld,bhndv->bhnlv', q_dec, S_prev)
    return (o_intra + o_inter).reshape(b, h, t, dv)


def moba_mixer(q, k, v):
    b, h, t, d = q.shape
    t_pad = -(-t // MOBA_BLOCK) * MOBA_BLOCK
    pad = ((0, 0), (0, 0), (0, t_pad - t), (0, 0))
    q, k, v = jnp.pad(q, pad), jnp.pad(k, pad), jnp.pad(v, pad)
    nb = t_pad // MOBA_BLOCK
    k_sel = min(MOBA_TOPK, nb)
    scale = d ** -0.5
    k_blocks = k.reshape(b, h, nb, MOBA_BLOCK, d)
    v_blocks = v.reshape(b, h, nb, MOBA_BLOCK, d)
    k_mean = jnp.mean(k_blocks, axis=3)
    pos = jnp.arange(t_pad)
    past = jnp.arange(nb)[None, :] < (pos // MOBA_BLOCK)[:, None]
    gate = jnp.einsum('bhtd,bhnd->bhtn', q, k_mean)
    gate = jnp.where(past, gate, -jnp.inf)
    top_val, top_idx = lax.top_k(gate, k_sel)
    valid = jnp.isfinite(top_val)
    bi = jnp.arange(b)[:, None, None, None]
    hi = jnp.arange(h)[None, :, None, None]
    qn = MOBA_QCHUNK

    def chunk_fn(c):
        start = c * qn
        qc = lax.dynamic_slice_in_dim(q, start, qn, axis=2)
        idx = lax.dynamic_slice_in_dim(top_idx, start, qn, axis=2)
        ok = lax.dynamic_slice_in_dim(valid, start, qn, axis=2)
        kg = k_blocks[bi, hi, idx]
        vg = v_blocks[bi, hi, idx]
        s_sel = jnp.einsum('bhqd,bhqskd->bhqsk', qc, kg) * scale
        s_sel = jnp.where(ok[..., None], s_sel, -jnp.inf).reshape(b, h, qn, k_sel * MOBA_BLOCK)
        own_start = (start // MOBA_BLOCK) * MOBA_BLOCK
        ko = lax.dynamic_slice_in_dim(k, own_start, MOBA_BLOCK, axis=2)
        vo = lax.dynamic_slice_in_dim(v, own_start, MOBA_BLOCK, axis=2)
        s_own = jnp.einsum('bhqd,bhkd->bhqk', qc, ko) * scale
        qpos = start + jnp.arange(qn)
        kpos = own_start + jnp.arange(MOBA_BLOCK)
        s_own = jnp.where(kpos[None, :] <= qpos[:, None], s_own, -jnp.inf)
        p = jax.nn.softmax(jnp.concatenate([s_sel, s_own], axis=-1).astype(jnp.float32), axis=-1)
        p_sel = p[..., :k_sel * MOBA_BLOCK].reshape(b, h, qn, k_sel, MOBA_BLOCK)
        p_own = p[..., k_sel * MOBA_BLOCK:]
        return (jnp.einsum('bhqsk,bhqskd->bhqd', p_sel, vg)
                + jnp.einsum('bhqk,bhkd->bhqd', p_own, vo))

    outs = lax.map(chunk_fn, jnp.arange(t_pad // qn))
    out = outs.transpose(1, 2, 0, 3, 4).reshape(b, h, t_pad, d)
    return out[:, :, :t]


def mlstm_mixer(q, k, v, log_i, log_f):
    b, h, t, dk = q.shape
    dv = v.shape[-1]
    L = MLSTM_CHUNK
    n = t // L
    qc = q.reshape(b, h, n, L, dk)
    kc = (k * dk ** -0.5).reshape(b, h, n, L, dk)
    vc = v.reshape(b, h, n, L, dv)
    li = log_i.reshape(b, h, n, L)
    bcum = jnp.cumsum(log_f.reshape(b, h, n, L), axis=-1)
    b_last = bcum[..., -1]
    causal = jnp.tril(jnp.ones((L, L), dtype=bool))
    logD = jnp.where(causal, bcum[..., :, None] - bcum[..., None, :] + li[..., None, :], -jnp.inf)
    log_end = b_last[..., None] - bcum + li
    m_local = jnp.max(log_end, axis=-1)
    w_loc = jnp.exp(log_end - m_local[..., None])
    chunk_kv = jnp.einsum('bhnl,bhnld,bhnlv->bhndv', w_loc, kc, vc)
    chunk_k = jnp.einsum('bhnl,bhnld->bhnd', w_loc, kc)

    def step(carry, inp):
        S, nv, m = carry
        bl, ml, ckv, ck = inp
        m_new = jnp.maximum(bl + m, ml)
        a = jnp.exp(bl + m - m_new)
        c = jnp.exp(ml - m_new)
        S_new = a[..., None, None] * S + c[..., None, None] * ckv
        n_new = a[..., None] * nv + c[..., None] * ck
        return (S_new, n_new, m_new), (S, nv, m)

    init = (jnp.zeros((b, h, dk, dv), q.dtype), jnp.zeros((b, h, dk), q.dtype), jnp.zeros((b, h), q.dtype))
    xs = (jnp.moveaxis(b_last, 2, 0), jnp.moveaxis(m_local, 2, 0),
          jnp.moveaxis(chunk_kv, 2, 0), jnp.moveaxis(chunk_k, 2, 0))
    _, (S_prev, n_prev, m_prev) = lax.scan(step, init, xs)
    S_prev = jnp.moveaxis(S_prev, 0, 2)
    n_prev = jnp.moveaxis(n_prev, 0, 2)
    m_prev = jnp.moveaxis(m_prev, 0, 2)
    m_inter = bcum + m_prev[..., None]
    m_row = jnp.maximum(m_inter, jnp.max(logD, axis=-1))
    s = jnp.einsum('bhnid,bhnjd->bhnij', qc, kc) * jnp.exp(logD - m_row[..., None])
    inter_w = jnp.exp(m_inter - m_row)
    num = (jnp.einsum('bhnij,bhnjv->bhniv', s, vc)
           + inter_w[..., None] * jnp.einsum('bhnid,bhndv->bhniv', qc, S_prev))
    den = jnp.sum(s, axis=-1) + inter_w * jnp.einsum('bhnid,bhnd->bhni', qc, n_prev)
    hid = num / jnp.maximum(jnp.abs(den), jnp.exp(-m_row))[..., None]
    return hid.reshape(b, h, t, dv)


def hybrid_layer(x, cos, sin, norm1_g, w_in, gla_a_up, gla_a_b, gla_norm_g, moba_qn_g, moba_kn_g,
                 mlstm_conv_w, mlstm_i_b, mlstm_f_b, mlstm_norm_g, gate_b, w_br_gla, w_br_moba,
                 w_br_mlstm, w_out, norm2_g, w_ff1, w_ff2):
    dt = x.dtype
    f32 = jnp.float32
    bsz, t, _ = x.shape
    hn = rms_norm(x, norm1_g)
    proj = hn @ w_in
    offsets = np.cumsum(IN_SPLITS)[:-1].tolist()
    (g_q, g_k, g_v, g_g, g_a, m_q, m_k, m_v,
     l_q, l_k, l_v, l_o, l_i, l_f, br_gate) = jnp.split(proj, offsets, axis=-1)

    log_a = jax.nn.log_sigmoid((g_a @ gla_a_up + gla_a_b).astype(f32)) / GLA_GATE_NORM
    o_gla = gla_mixer(split_heads(g_q.astype(f32), GLA_HEADS), split_heads(g_k.astype(f32), GLA_HEADS),
                      split_heads(g_v.astype(f32), GLA_HEADS), split_heads(log_a, GLA_HEADS))
    o_gla = rms_norm(o_gla, gla_norm_g)
    y_gla = (merge_heads(o_gla) * jax.nn.silu(g_g.astype(f32))).astype(dt)

    mq = apply_rope(rms_norm(split_heads(m_q.astype(f32), MOBA_HEADS), moba_qn_g), cos, sin)
    mk = apply_rope(rms_norm(split_heads(m_k.astype(f32), MOBA_HEADS), moba_kn_g), cos, sin)
    mv = split_heads(m_v.astype(f32), MOBA_HEADS)
    y_moba = merge_heads(moba_mixer(mq, mk, mv)).astype(dt)

    qk = causal_dwconv(jnp.concatenate([l_q, l_k], axis=-1).astype(f32), mlstm_conv_w.astype(f32))
    lq, lk = jnp.split(qk, 2, axis=-1)
    log_i = (l_i + mlstm_i_b).astype(f32).transpose(0, 2, 1)
    log_f = jax.nn.log_sigmoid((l_f + mlstm_f_b).astype(f32)).transpose(0, 2, 1)
    h_ml = mlstm_mixer(split_heads(lq, MLSTM_HEADS), split_heads(lk, MLSTM_HEADS),
                       split_heads(l_v.astype(f32), MLSTM_HEADS), log_i, log_f)
    h_ml = rms_norm(h_ml, mlstm_norm_g)
    y_mlstm = (merge_heads(h_ml) * jax.nn.sigmoid(l_o.astype(f32))).astype(dt)

    gates = jax.nn.sigmoid(br_gate + gate_b).reshape(bsz, t, N_BRANCH, D_MODEL)
    mixed = (gates[:, :, 0] * (y_gla @ w_br_gla)
             + gates[:, :, 1] * (y_moba @ w_br_moba)
             + gates[:, :, 2] * (y_mlstm @ w_br_mlstm))
    x = x + mixed @ w_out

    h2 = rms_norm(x, norm2_g)
    x = x + jnp.square(jax.nn.relu(h2 @ w_ff1)) @ w_ff2
    return x


def setup_inputs(seed: int = 0) -> dict:
    key = jax.random.key(seed)
    ks = jax.random.split(key, 24)

    def nrm(k, shape, fan_in):
        return jax.random.normal(k, shape, jnp.float32) * (fan_in ** -0.5)

    def gain(k, shape):
        return 1.0 + 0.02 * jax.random.normal(k, shape, jnp.float32)

    f_bias = (jnp.linspace(3.0, 6.0, MLSTM_HEADS, dtype=jnp.float32)[None, :]
              + 0.1 * jax.random.normal(ks[10], (DEPTH, MLSTM_HEADS), jnp.float32))
    return {
        "x": jax.random.normal(ks[0], (BATCH, SEQ, D_MODEL), jnp.float32),
        "norm1_g": gain(ks[1], (DEPTH, D_MODEL)),
        "w_in": nrm(ks[2], (DEPTH, D_MODEL, D_IN), D_MODEL),
        "gla_a_up": nrm(ks[3], (DEPTH, GLA_LOWRANK, GLA_K), GLA_LOWRANK),
        "gla_a_b": 0.02 * jax.random.normal(ks[4], (DEPTH, GLA_K), jnp.float32),
        "gla_norm_g": gain(ks[5], (DEPTH, GLA_DV)),
        "moba_qn_g": gain(ks[6], (DEPTH, MOBA_HD)),
        "moba_kn_g": gain(ks[7], (DEPTH, MOBA_HD)),
        "mlstm_conv_w": nrm(ks[8], (DEPTH, MLSTM_CONV, 2 * MLSTM_QK), MLSTM_CONV),
        "mlstm_i_b": 0.1 * jax.random.normal(ks[9], (DEPTH, MLSTM_HEADS), jnp.float32),
        "mlstm_f_b": f_bias,
        "mlstm_norm_g": gain(ks[11], (DEPTH, MLSTM_DV)),
        "gate_b": 0.02 * jax.random.normal(ks[12], (DEPTH, N_BRANCH * D_MODEL), jnp.float32),
        "w_br_gla": nrm(ks[13], (DEPTH, GLA_V, D_MODEL), GLA_V),
        "w_br_moba": nrm(ks[14], (DEPTH, MOBA_W, D_MODEL), MOBA_W),
        "w_br_mlstm": nrm(ks[15], (DEPTH, MLSTM_V, D_MODEL), MLSTM_V),
        "w_out": nrm(ks[16], (DEPTH, D_MODEL, D_MODEL), D_MODEL),
        "norm2_g": gain(ks[17], (DEPTH, D_MODEL)),
        "w_ff1": nrm(ks[18], (DEPTH, D_MODEL, D_FF), D_MODEL),
        "w_ff2": nrm(ks[19], (DEPTH, D_FF, D_MODEL), D_FF),
    }


def reference(x, norm1_g, w_in, gla_a_up, gla_a_b, gla_norm_g, moba_qn_g, moba_kn_g, mlstm_conv_w,
              mlstm_i_b, mlstm_f_b, mlstm_norm_g, gate_b, w_br_gla, w_br_moba, w_br_mlstm, w_out,
              norm2_g, w_ff1, w_ff2):
    cos, sin = rope_tables(x.shape[1], MOBA_HD)
    for l in range(DEPTH):
        x = hybrid_layer(x, cos, sin, norm1_g[l], w_in[l], gla_a_up[l], gla_a_b[l], gla_norm_g[l],
                         moba_qn_g[l], moba_kn_g[l], mlstm_conv_w[l], mlstm_i_b[l], mlstm_f_b[l],
                         mlstm_norm_g[l], gate_b[l], w_br_gla[l], w_br_moba[l], w_br_mlstm[l], w_out[l],
                         norm2_g[l], w_ff1[l], w_ff2[l])
    return x
```

```python
import numpy as np
import concourse.bass as bass
import concourse.mybir as mybir
from contextlib import ExitStack

F32 = mybir.dt.float32
BF16 = mybir.dt.bfloat16
AF = mybir.ActivationFunctionType
ALU = mybir.AluOpType
AX = mybir.AxisListType


class Buf:
    __slots__ = ("ap", "w", "r", "dsem", "dcnt", "name", "excl")

    def __init__(self, ap, name=""):
        self.ap = ap
        self.w = {}
        self.r = {}
        self.dsem = None
        self.dcnt = 0
        self.name = name
        self.excl = False

    def __getitem__(self, idx):
        return self.ap[idx]


class EngW:
    def __init__(self, S, name):
        self.S = S
        self.name = name
        self.e = getattr(S.nc, name)
        self.sem = S.stack.enter_context(S.nc.semaphore("es_" + name))
        self.cnt = 0
        self.waited = {}

    def wait(self, ev):
        if ev is None:
            return
        sem, val = ev
        k = id(sem)
        if self.waited.get(k, 0) >= val:
            return
        self.e.wait_ge(sem, val)
        self.waited[k] = val


class Sched:
    def __init__(self, nc, stack):
        self.nc = nc
        self.stack = stack
        self.E = {n: EngW(self, n) for n in ("tensor", "vector", "scalar", "gpsimd", "sync")}
        self.nsem = 5
        self.pending = {}

    def sbuf(self, name, shape, dt):
        t = self.stack.enter_context(self.nc.sbuf_tensor(name, list(shape), dt))
        return t

    def psum(self, name, shape, dt=F32):
        t = self.stack.enter_context(self.nc.psum_tensor(name, list(shape), dt))
        return t

    def buf(self, ap, name=""):
        return Buf(ap, name)

    def newsem(self, name):
        self.nsem += 1
        return self.stack.enter_context(self.nc.semaphore(name))

    def _deps(self, E, reads, writes, pwrites, same_ok, skip_sem=None):
        for b in reads:
            for ev in b.w.values():
                E.wait(ev)
            if b.excl:
                for ev in b.r.values():
                    if ev[0] is not E.sem:
                        E.wait(ev)
        for b in writes:
            for ev in b.w.values():
                if not (same_ok and ev[0] is E.sem):
                    E.wait(ev)
            for ev in b.r.values():
                if not (same_ok and ev[0] is E.sem):
                    E.wait(ev)
        for b in pwrites:
            for ev in b.w.values():
                if ev[0] is not E.sem and ev[0] is not skip_sem:
                    E.wait(ev)
            for ev in b.r.values():
                if not (same_ok and ev[0] is E.sem):
                    E.wait(ev)

    def _record(self, ev, reads, writes, pwrites):
        k = id(ev[0])
        for b in reads:
            b.r[k] = ev
        for b in writes:
            b.w = {k: ev}
            b.r = {}
        for b in pwrites:
            b.w[k] = ev

    def op(self, eng, fn, reads=(), writes=(), pwrites=(), inc=True):
        E = self.E[eng]
        self._deps(E, reads, writes, pwrites, eng == "tensor")
        ins = fn(E.e)
        if inc:
            E.cnt += 1
            ins.then_inc(E.sem, 1)
            ev = (E.sem, E.cnt)
        else:
            ev = (E.sem, E.cnt + 1)
        self._record(ev, reads, writes, pwrites)
        return ev

    def dma(self, q, out_ap, in_ap, reads=(), writes=(), pwrites=(), sem_owner=None, **kw):
        E = self.E[q]
        owner = sem_owner or (writes[0] if writes else (pwrites[0] if pwrites else reads[0]))
        if owner.dsem is None:
            owner.dsem = self.newsem("ds_%d" % self.nsem)
        self._deps(E, reads, writes, pwrites, False, skip_sem=owner.dsem)
        ins = E.e.dma_start(out=out_ap, in_=in_ap, **kw)
        owner.dcnt += 16
        ins.then_inc(owner.dsem, 16)
        ev = (owner.dsem, owner.dcnt)
        self._record(ev, reads, writes, pwrites)
        return ev

    def finish(self, bufs, eng="sync"):
        E = self.E[eng]
        for b in bufs:
            for ev in b.w.values():
                E.wait(ev)
            for ev in b.r.values():
                E.wait(ev)


EPS = 1e-6


def emit_norm(S, K, x_sb, xb, g_sb, gb, out_sb, outb, ntok, ps_ss, ps_ssb, sq_sb, sqb, tmp_sb, tmpb):
    nc = S.nc
    for h in range(ntok // 512):
        hs = slice(h * 512, (h + 1) * 512)
        S.op("scalar", lambda e: e.activation(out=sq_sb[:, :, :], in_=x_sb[:, :, hs], func=AF.Square),
             reads=[xb], writes=[sqb])
        for c in range(8):
            S.op("tensor", lambda e: e.matmul(ps_ss[:, :], lhsT=K["ones"][:, :], rhs=sq_sb[:, c, :],
                                              start=(c == 0), stop=(c == 7)),
                 reads=[K["onesb"], sqb], writes=[ps_ssb], inc=(c == 7))
        S.op("scalar", lambda e: e.activation(out=tmp_sb[:, :], in_=ps_ss[:, :], func=AF.Ln, scale=1.0 / 1024, bias=K["eps"][:, 0:1]),
             reads=[ps_ssb, K["epsb"]], writes=[tmpb])
        S.op("scalar", lambda e: e.activation(out=tmp_sb[:, :], in_=tmp_sb[:, :], func=AF.Exp, scale=-0.5),
             reads=[tmpb], writes=[tmpb])
        for c in range(8):
            S.op("vector", lambda e: e.scalar_tensor_tensor(out=out_sb[:, c, hs], in0=x_sb[:, c, hs], scalar=g_sb[:, c:c + 1],
                                                            in1=tmp_sb[:, :], op0=ALU.mult, op1=ALU.mult),
                 reads=[xb, gb, tmpb], pwrites=[outb])


def build_p3(ntok, last, first_only=False, stop_after='Z'):
    nc = bass.Bass("TRN2", target_bir_lowering=False)
    D = lambda name, shape, dt=F32, kind="ExternalInput": nc.dram_tensor(name, list(shape), dt, kind=kind).ap()
    xT = D("xT", [1024, ntok])
    g1n = D("g1n", [128, 8])
    if not first_only:
        hnT = D("hnT", [1024, ntok], BF16)
        yT = D("yT", [1536, ntok], BF16)
        wg = D("wg", [24, 128, 8, 128]); gbias = D("gbias", [128, 24])
        wbr = D("wbr", [24, 128, 4, 128])
        wo = D("wo", [8, 128, 8, 128])
        g2 = D("g2", [128, 8])
        w1 = D("w1", [32, 128, 8, 128]); w2 = D("w2", [8, 128, 32, 128])
        xo = D("xo", [1024, ntok], F32, "ExternalOutput")
    if not last:
        hno = D("hno", [1024, ntok], BF16, "ExternalOutput")
    G = 1024
    with ExitStack() as st:
        S = Sched(nc, st)
        K = {}
        K["ones"] = S.sbuf("ones", [128, 128], BF16); K["onesb"] = S.buf(K["ones"])
        K["eps"] = S.sbuf("epsc", [128, 1], F32); K["epsb"] = S.buf(K["eps"])
        S.op("vector", lambda e: e.memset(K["ones"][:, :], 1.0), writes=[K["onesb"]])
        S.op("vector", lambda e: e.memset(K["eps"][:, :], EPS), writes=[K["epsb"]])
        x_sb = S.sbuf("x_sb", [128, 8, G], F32); xb = S.buf(x_sb)
        hn_sb = S.sbuf("hn_sb", [128, 8, G], BF16); hnb = S.buf(hn_sb)
        g1_sb = S.sbuf("g1_sb", [128, 8], F32); g1b = S.buf(g1_sb)
        sq_sb = S.sbuf("sq_sb", [128, 8, 512], BF16); sqb = S.buf(sq_sb)
        tmp_sb = S.sbuf("tmp_sb", [128, 512], F32); tmpb = S.buf(tmp_sb)
        PS = [S.psum("ps%d" % i, [128, 512]) for i in range(8)]
        PSb = [S.buf(p) for p in PS]
        S.dma("sync", g1_sb[:, :], g1n, writes=[g1b])
        xTr = xT.rearrange("(c p) t -> p c t", p=128)
        if not last:
            hnor = hno.rearrange("(c p) t -> p c t", p=128)
            hnob = S.buf(hno)
        if first_only:
            for gi in range(ntok // G):
                gs = slice(gi * G, (gi + 1) * G)
                S.dma("sync", x_sb[:, :, :], xTr[:, :, gs], writes=[xb])
                emit_norm(S, K, x_sb, xb, g1_sb, g1b, hn_sb, hnb, G, PS[0], PSb[0], sq_sb, sqb, tmp_sb, tmpb)
                S.dma("sync", hnor[:, :, gs], hn_sb[:, :, :], reads=[hnb], pwrites=[hnob], sem_owner=hnb)
            S.finish([hnob])
            return nc
        mixed = S.sbuf("mixed", [128, 8, G], BF16); mixb = S.buf(mixed)
        big = S.sbuf("big", [128, 32, G], BF16); bigb = S.buf(big)
        NW = 3
        wt = [S.sbuf("wt%d" % i, [128, 32 * 128], BF16) for i in range(NW)]
        wtb = [S.buf(w) for w in wt]
        wctr = [0]
        gb_sb = S.sbuf("gb_sb", [128, 24], F32); gbb = S.buf(gb_sb)
        g2_sb = S.sbuf("g2_sb", [128, 8], F32); g2b = S.buf(g2_sb)
        acc = S.sbuf("acc", [128, G], F32); accb = S.buf(acc)
        t1 = [S.sbuf("t1_%d" % i, [128, 512], F32) for i in range(2)]; t1b = [S.buf(t) for t in t1]
        S.dma("sync", gb_sb[:, :], gbias, writes=[gbb])
        S.dma("sync", g2_sb[:, :], g2, writes=[g2b])
        S.op("vector", lambda e: e.tensor_scalar(out=gb_sb[:, :], in0=gb_sb[:, :], scalar1=-1.0, scalar2=None, op0=ALU.mult),
             reads=[gbb], writes=[gbb])
        xob = S.buf(xo)
        xor_ = xo.rearrange("(c p) t -> p c t", p=128)
        hnTr = hnT.rearrange("(c p) t -> p c t", p=128)
        yTr = yT.rearrange("(c p) t -> p c t", p=128)

        def wtile(src, kc):
            i = wctr[0] % NW
            wctr[0] += 1
            view = wt[i][:, 0:kc * 128].rearrange("p (k n) -> p k n", n=128)
            S.dma("gpsimd", view, src, writes=[wtb[i]])
            return view, wtb[i]

        psi = [0]

        def nextps():
            i = psi[0] % 6
            psi[0] += 1
            return PS[i], PSb[i]

        tci = [0]
        for gi in range(ntok // G):
            gs = slice(gi * G, (gi + 1) * G)
            S.dma("sync", x_sb[:, :, :], xTr[:, :, gs], writes=[xb])
            S.dma("sync", hn_sb[:, :, :], hnTr[:, :, gs], writes=[hnb])
            S.dma("sync", big[:, 0:12, :], yTr[:, :, gs], writes=[bigb])
            for m in range(8 if stop_after >= 'B' else 0):
                for br in range(3):
                    j = br * 8 + m
                    wgt, wgtb = wtile(wg[j], 8)
                    wbt, wbtb = wtile(wbr[j], 4)
                    for h in range(2):
                        hs = slice(h * 512, (h + 1) * 512)
                        pg, pgb = nextps()
                        for c in range(8):
                            S.op("tensor", lambda e: e.matmul(pg[:, :], lhsT=wgt[:, c, :], rhs=hn_sb[:, c, hs], start=(c == 0), stop=(c == 7)),
                                 reads=[wgtb, hnb], writes=[pgb], inc=(c == 7))
                        pb, pbb = nextps()
                        for g in range(4):
                            S.op("tensor", lambda e: e.matmul(pb[:, :], lhsT=wbt[:, g, :], rhs=big[:, g * 3 + br, hs], start=(g == 0), stop=(g == 3)),
                                 reads=[wbtb, bigb], writes=[pbb], inc=(g == 3))
                        ti = tci[0] % 2; tci[0] += 1
                        tt, ttb = t1[ti], t1b[ti]
                        S.op("scalar", lambda e: e.activation(out=tt[:, :], in_=pg[:, :], func=AF.Exp, scale=-1.0, bias=gb_sb[:, j:j + 1]),
                             reads=[pgb, gbb], writes=[ttb])
                        S.op("scalar", lambda e: e.activation(out=tt[:, :], in_=tt[:, :], func=AF.Ln, bias=1.0),
                             reads=[ttb], writes=[ttb])
                        S.op("scalar", lambda e: e.activation(out=tt[:, :], in_=tt[:, :], func=AF.Exp, scale=-1.0),
                             reads=[ttb], writes=[ttb])
                        if br == 0:
                            S.op("vector", lambda e: e.tensor_tensor(out=acc[:, hs], in0=tt[:, :], in1=pb[:, :], op=ALU.mult),
                                 reads=[ttb, pbb], pwrites=[accb])
                        else:
                            S.op("vector", lambda e: e.tensor_tensor(out=tt[:, :], in0=tt[:, :], in1=pb[:, :], op=ALU.mult),
                                 reads=[ttb, pbb], writes=[ttb])
                            if br == 1:
                                S.op("vector", lambda e: e.tensor_tensor(out=acc[:, hs], in0=acc[:, hs], in1=tt[:, :], op=ALU.add),
                                     reads=[ttb, accb], pwrites=[accb])
                            else:
                                S.op("vector", lambda e: e.tensor_tensor(out=mixed[:, m, hs], in0=acc[:, hs], in1=tt[:, :], op=ALU.add),
                                     reads=[ttb, accb], pwrites=[mixb])
            for m in range(8 if stop_after >= 'C' else 0):
                wot, wotb = wtile(wo[m], 8)
                for h in range(2):
                    hs = slice(h * 512, (h + 1) * 512)
                    p, pb_ = nextps()
                    for c in range(8):
                        S.op("tensor", lambda e: e.matmul(p[:, :], lhsT=wot[:, c, :], rhs=mixed[:, c, hs], start=(c == 0), stop=(c == 7)),
                             reads=[wotb, mixb], writes=[pb_], inc=(c == 7))
                    S.op("vector", lambda e: e.tensor_tensor(out=x_sb[:, m, hs], in0=x_sb[:, m, hs], in1=p[:, :], op=ALU.add),
                         reads=[pb_, xb], pwrites=[xb])
            emit_norm(S, K, x_sb, xb, g2_sb, g2b, hn_sb, hnb, G, PS[6], PSb[6], sq_sb, sqb, tmp_sb, tmpb)
            for f in range(32 if stop_after >= 'E' else 0):
                w1t, w1tb = wtile(w1[f], 8)
                for h in range(2):
                    hs = slice(h * 512, (h + 1) * 512)
                    p, pb_ = nextps()
                    for c in range(8):
                        S.op("tensor", lambda e: e.matmul(p[:, :], lhsT=w1t[:, c, :], rhs=hn_sb[:, c, hs], start=(c == 0), stop=(c == 7)),
                             reads=[w1tb, hnb], writes=[pb_], inc=(c == 7))
                    ti = tci[0] % 2; tci[0] += 1
                    tt, ttb = t1[ti], t1b[ti]
                    S.op("scalar", lambda e: e.activation(out=tt[:, :], in_=p[:, :], func=AF.Relu), reads=[pb_], writes=[ttb])
                    S.op("vector", lambda e: e.tensor_tensor(out=big[:, f, hs], in0=tt[:, :], in1=tt[:, :], op=ALU.mult),
                         reads=[ttb], pwrites=[bigb])
            for m in range(8 if stop_after >= 'F' else 0):
                w2t, w2tb = wtile(w2[m], 32)
                for h in range(2):
                    hs = slice(h * 512, (h + 1) * 512)
                    p, pb_ = nextps()
                    for f in range(32):
                        S.op("tensor", lambda e: e.matmul(p[:, :], lhsT=w2t[:, f, :], rhs=big[:, f, hs], start=(f == 0), stop=(f == 31)),
                             reads=[w2tb, bigb], writes=[pb_], inc=(f == 31))
                    S.op("vector", lambda e: e.tensor_tensor(out=x_sb[:, m, hs], in0=x_sb[:, m, hs], in1=p[:, :], op=ALU.add),
                         reads=[pb_, xb], pwrites=[xb])
            S.dma("sync", xor_[:, :, gs], x_sb[:, :, :], reads=[xb], pwrites=[xob], sem_owner=xb)
            if not last:
                emit_norm(S, K, x_sb, xb, g1_sb, g1b, hn_sb, hnb, G, PS[6], PSb[6], sq_sb, sqb, tmp_sb, tmpb)
                S.dma("sync", hnor[:, :, gs], hn_sb[:, :, :], reads=[hnb], pwrites=[hnob], sem_owner=hnb)
        S.finish([xob] + ([hnob] if not last else []))
    return nc


EPS = 1e-6
NEG = -30000.0
NCOL = 1298
MQK_LEVEL = [99]
GL = [99]
TM_LEVEL = [99]
STAGES = ['gd', 'mqk', 'gate', 'mlc', 'tm', 'gla', 'ml', 'moba', 'out']
C_GQ, C_GK, C_GA, C_MQA, C_MQB, C_MKA, C_MKB, C_LQ, C_LK = 0, 128, 256, 272, 336, 400, 464, 528, 592
C_TM1, C_TM2 = 656, 1040


def build_p2(T):
    nc = bass.Bass("TRN2", target_bir_lowering=False)
    D = lambda name, shape, dt=F32, kind="ExternalInput": nc.dram_tensor(name, list(shape), dt, kind=kind).ap()
    NT = T // 512
    NKT = T // 128
    NB = T // 256
    hnT = D("hnT", [1024, T], BF16)
    w = D("w", [128, 8, NCOL])
    pp = D("pp", [128, 16])
    bc = D("bc", [128, 258])
    up = D("up", [16, 128])
    cosT = D("cosT", [64, T]); sinT = D("sinT", [64, T])
    cst = D("cst", [128, 1472])
    onehot = D("onehot", [32, T])
    yT = D("yT", [384, T], BF16, "ExternalOutput")
    with ExitStack() as st:
        S = Sched(nc, st)
        sb = lambda name, shape, dt=F32: S.sbuf(name, shape, dt)
        cf = sb("cf", [128, 1472]); cfb = S.buf(cf)
        S.dma("sync", cf[:, :], cst[:, 0:1472], writes=[cfb])
        mask_f = cf[:, 0:128]; SL_f = cf[:, 128:256]; ident_f = cf[:, 256:384]; reset_f = cf[:, 384:896]
        cb16 = sb("cb16", [128, 1472], BF16); cbb = S.buf(cb16)
        S.op("vector", lambda e: e.tensor_copy(out=cb16[:, :], in_=cf[:, :]), reads=[cfb], writes=[cbb])
        mask_b = cb16[:, 0:128]; ident_b = cb16[:, 256:384]; CBd = cb16[:, 896:1408]; RT = cb16[0:64, 1408:1472]
        ones_b = sb("ones_b", [128, 128], BF16); onb = S.buf(ones_b)
        S.op("vector", lambda e: e.memset(ones_b[:, :], 1.0), writes=[onb])
        ones_f = sb("ones_f", [128, 64]); onfb = S.buf(ones_f)
        S.op("vector", lambda e: e.memset(ones_f[:, :], 1.0), writes=[onfb])
        epsc = sb("epsc", [128, 1]); epsb = S.buf(epsc)
        S.op("vector", lambda e: e.memset(epsc[:, :], EPS), writes=[epsb])
        pp_sb = sb("pp_sb", [128, 16]); ppb = S.buf(pp_sb)
        S.dma("sync", pp_sb[:, :], pp, writes=[ppb])
        bc_sb = sb("bc_sb", [128, 258]); bcb = S.buf(bc_sb)
        S.dma("sync", bc_sb[:, :], bc, writes=[bcb])
        neg_sb = sb("neg_sb", [128, 2]); negb_ = S.buf(neg_sb)
        S.op("vector", lambda e: e.tensor_scalar(out=neg_sb[:, 0:1], in0=pp_sb[:, 0:1], scalar1=-1.0, scalar2=None, op0=ALU.mult),
             reads=[ppb], pwrites=[negb_])
        S.op("vector", lambda e: e.tensor_scalar(out=neg_sb[:, 1:2], in0=bc_sb[:, 257:258], scalar1=-1.0, scalar2=None, op0=ALU.mult),
             reads=[bcb], pwrites=[negb_])
        ppc = sb("ppc", [128, 16]); ppcb = S.buf(ppc)
        S.op("vector", lambda e: e.tensor_copy(out=ppc[:, :], in_=pp_sb[:, :]), reads=[ppb], writes=[ppcb])
        S.op("vector", lambda e: e.tensor_scalar(out=ppc[:, 7:11], in0=pp_sb[:, 7:11], scalar1=0.125, scalar2=None, op0=ALU.mult), reads=[ppb], writes=[ppcb])
        up_sb = sb("up_sb", [16, 128], BF16); upb = S.buf(up_sb)
        S.dma("gpsimd", up_sb[:, :], up, writes=[upb])
        w_sb = sb("w_sb", [128, 8, NCOL], BF16); wb = S.buf(w_sb)
        for c in range(8):
            S.dma("gpsimd", w_sb[:, c, :], w[:, c, :], pwrites=[wb], sem_owner=wb)
        kaug = [sb("kaug%d" % h, [96, T], BF16) for h in range(2)]; kaugb = [S.buf(k) for k in kaug]
        for h in range(2):
            S.dma("gpsimd", kaug[h][64:96, :], onehot, pwrites=[kaugb[h]], sem_owner=kaugb[h])
        qaug = [sb("qaug%d" % h, [96, 512], BF16) for h in range(2)]; qaugb = [S.buf(q) for q in qaug]
        Vst = sb("Vst", [128, NKT, 2, 65], BF16); Vstb = S.buf(Vst)
        S.op("gpsimd", lambda e: e.memset(Vst[:, :, :, :], 1.0), writes=[Vstb])
        KmT = sb("KmT", [64, 2, 32], BF16); KmTb = S.buf(KmT)
        S.op("vector", lambda e: e.memset(KmT[:, :, :], 0.0), writes=[KmTb])
        Wst = [sb("Wst%d" % i, [128, 128], BF16) for i in range(2)]; Wstb = [S.buf(x) for x in Wst]
        S.op("vector", lambda e: e.memset(Wst[0][:, :], 0.0), writes=[Wstb[0]])
        dprev = sb("dprev", [128, 1]); dprevb = S.buf(dprev)
        S.op("vector", lambda e: e.memset(dprev[:, :], 1.0), writes=[dprevb])
        C32 = sb("C32", [64, 129]); C32b = S.buf(C32)
        Cbf = sb("Cbf", [64, 129], BF16); Cbfb = S.buf(Cbf)
        S.op("vector", lambda e: e.memset(C32[:, :], 0.0), writes=[C32b])
        S.op("vector", lambda e: e.memset(Cbf[:, :], 0.0), writes=[Cbfb])
        xq = [sb("xq%d" % i, [64, 515]) for i in range(2)]; xqb = [S.buf(x) for x in xq]
        for i in range(2):
            S.op("vector", lambda e: e.memset(xq[i][:, :], 0.0), writes=[xqb[i]])
        mb96 = sb("mb96", [128, 96], BF16); mb96b = S.buf(mb96)
        S.op("vector", lambda e: e.memset(mb96[:, :], 0.0), writes=[mb96b])
        hn_sb = [sb("hn%d" % i, [128, 8, 512], BF16) for i in range(2)]; hnb = [S.buf(x) for x in hn_sb]
        cs_sb = sb("cos_sb", [64, 2, 512]); csb = S.buf(cs_sb)
        ga_sb = sb("ga_sb", [16, 512], BF16); gab = S.buf(ga_sb)
        e1 = sb("e1", [128, 512]); e1b = S.buf(e1)
        eb = sb("eb", [128, 512]); ebb = S.buf(eb)
        enb = sb("enb", [128, 512]); enbb = S.buf(enb)
        qdec = sb("qdec", [128, 512], BF16); qdecb = S.buf(qdec)
        q2 = sb("q2", [128, 512], BF16); q2b = S.buf(q2)
        kinv = sb("kinv", [128, 512], BF16); kinvb = S.buf(kinv)
        sq = sb("sq", [64, 512], BF16); sqb = S.buf(sq)
        rs = sb("rs", [64, 512]); rsb = S.buf(rs)
        kg = sb("kg", [64, 512], BF16); kgb = S.buf(kg)
        ta = sb("ta", [64, 512]); tab = S.buf(ta)
        tb = sb("tb", [64, 512]); tbb = S.buf(tb)
        km = sb("km", [64, 2]); kmb = S.buf(km)
        gsb = sb("gsb", [128, 32]); gsbb = S.buf(gsb)
        mx8 = sb("mx8", [128, 8]); mx8b = S.buf(mx8)
        cva = sb("cva", [64, 512]); cvab = S.buf(cva)
        qc = sb("qc", [64, 512], BF16); qcb = S.buf(qc)
        kc = sb("kc", [64, 512], BF16); kcb = S.buf(kc)
        vtm = sb("vtm", [128, 4, 384], BF16); vtmb = S.buf(vtm)
        vml = sb("vml", [128, 4, 129], BF16); vmlb = S.buf(vml)
        S.op("vector", lambda e: e.memset(vml[:, :, :], 1.0), writes=[vmlb])
        ee = sb("ee", [128, 256]); eeb = S.buf(ee)
        gmo = sb("gmo", [128, 4, 128]); gmob = S.buf(gmo)
        gmg = sb("gmg", [128, 4, 128]); gmgb = S.buf(gmg)
        li = sb("li", [128, 4]); lib = S.buf(li)
        nlf = sb("nlf", [128, 4]); nlfb = S.buf(nlf)
        ytok = sb("ytok", [128, 4, 384], BF16); ytokb = S.buf(ytok)
        yT_sb = sb("yT_sb", [128, 3, 512], BF16); yTb = S.buf(yT_sb)
        Pb = [sb("P%d" % i, [128, 512], BF16) for i in range(2)]; Pbb = [S.buf(x) for x in Pb]
        kinvT = sb("kinvT", [128, 128], BF16); kinvTb = S.buf(kinvT)
        attn = sb("attn", [128, 128], BF16); attnb = S.buf(attn)
        junk = sb("junk", [128, 129], BF16); junkb = S.buf(junk)
        st1 = sb("st1", [128, 4]); st1b = S.buf(st1)
        Am = sb("Am", [128, 128]); Amb = S.buf(Am)
        DT = sb("DT", [128, 128]); DTb = S.buf(DT)
        DTm = sb("DTm", [128, 128]); DTmb = S.buf(DTm)
        sT = sb("sT", [128, 128], BF16); sTb = S.buf(sT)
        tot = sb("tot", [128, 129]); totb = S.buf(tot)
        hh = sb("hh", [128, 128]); hhb = S.buf(hh)
        kw = sb("kw", [128, 64], BF16); kwb = S.buf(kw)
        nbc = sb("nbc", [128, 64]); nbcb = S.buf(nbc)
        efl = sb("efl", [64, 1]); eflb = S.buf(efl)
        rec4 = sb("rec4", [128, 4]); rec4b = S.buf(rec4)
        accs = sb("accs", [128, 260]); accsb = S.buf(accs)
        obg = sb("obg", [128, 4, 128], BF16); obgb = S.buf(obg)
        ubg = sb("ubg", [128, 4, 128], BF16); ubgb = S.buf(ubg)
        obm = sb("obm", [128, 4, 128], BF16); obmb = S.buf(obm)
        ubm = sb("ubm", [128, 4, 128], BF16); ubmb = S.buf(ubm)
        osq = sb("osq", [128, 128], BF16); osqb = S.buf(osq)
        rsn = sb("rsn", [128, 128]); rsnb = S.buf(rsn)
        PS = [S.psum("ps%d" % i, [128, 512]) for i in range(7)]; PSb = [S.buf(p) for p in PS]
        pst = S.psum("pst", [128, 1024], BF16); pstb = S.buf(pst)
        for b_ in PSb + [pstb]:
            b_.excl = True
        rot = [0]

        def nextps():
            i = 4 + rot[0] % 3
            rot[0] += 1
            return PS[i], PSb[i]
        fmi = [0]

        def fmps():
            i = fmi[0] % 2
            fmi[0] += 1
            return PS[i], PSb[i]

        V = lambda fn, r=(), w=(), pw=(): S.op("vector", fn, reads=r, writes=w, pwrites=pw)
        A = lambda fn, r=(), w=(), pw=(): S.op("scalar", fn, reads=r, writes=w, pwrites=pw)
        G = lambda fn, r=(), w=(), pw=(): S.op("gpsimd", fn, reads=r, writes=w, pwrites=pw)
        MM = lambda fn, r=(), w=(), pw=(), inc=True: S.op("tensor", fn, reads=r, writes=w, pwrites=pw, inc=inc)

        hnTr = hnT.rearrange("(c p) t -> p c t", p=128)
        yTr = yT.rearrange("(c p) t -> p c t", p=128)
        yTdb = S.buf(yT)

        def inproj_fm(hs, hsb, col, M):
            p, pb = fmps()
            for c in range(8):
                MM(lambda e: e.matmul(p[0:M, :], lhsT=w_sb[:, c, col:col + M], rhs=hs[:, c, :], start=(c == 0), stop=(c == 7)),
                   r=[wb, hsb], w=[pb], inc=(c == 7))
            return p, pb

        def rsqrt_small(dst, dstb, src, srcb, n, scale):
            A(lambda e: e.activation(out=dst, in_=src, func=AF.Ln, scale=scale, bias=epsc[0:n, 0:1]), r=[srcb, epsb], w=[dstb])
            A(lambda e: e.activation(out=dst, in_=dst, func=AF.Exp, scale=-0.5), r=[dstb], w=[dstb])

        for t in range(NT):
            ts = t * 512
            hs, hsb = hn_sb[t % 2], hnb[t % 2]
            S.dma("sync", hs[:, :, :], hnTr[:, :, ts:ts + 512], writes=[hsb])
            S.dma("sync", cs_sb[:, 0, :], cosT[:, ts:ts + 512], pwrites=[csb], sem_owner=csb)
            S.dma("sync", cs_sb[:, 1, :], sinT[:, ts:ts + 512], pwrites=[csb], sem_owner=csb)
            if 'gd' in STAGES:
                p, pb = inproj_fm(hs, hsb, C_GA, 16)
                A(lambda e: e.copy(out=ga_sb[:, :], in_=p[0:16, :]), r=[pb], w=[gab])
                pz, pzb = nextps()
                MM(lambda e: e.matmul(pz[:, :], lhsT=up_sb[:, :], rhs=ga_sb[:, :], start=True, stop=True), r=[upb, gab], w=[pzb])
                A(lambda e: e.activation(out=e1[:, :], in_=pz[:, :], func=AF.Exp, scale=-1.0, bias=neg_sb[:, 0:1]), r=[pzb, negb_], w=[e1b])
                A(lambda e: e.activation(out=e1[:, :], in_=e1[:, :], func=AF.Ln, bias=1.0), r=[e1b], w=[e1b])
                V(lambda e: e.tensor_tensor_scan(out=e1[:, :], data0=reset_f, data1=e1[:, :], initial=0.0, op0=ALU.mult, op1=ALU.add),
                  r=[e1b, cfb], w=[e1b])
                A(lambda e: e.activation(out=eb[:, :], in_=e1[:, :], func=AF.Exp, scale=-1.0 / 16), r=[e1b], w=[ebb])
                A(lambda e: e.activation(out=enb[:, :], in_=e1[:, :], func=AF.Exp, scale=1.0 / 16), r=[e1b], w=[enbb])
                p, pb = inproj_fm(hs, hsb, C_GQ, 128)
                V(lambda e: e.scalar_tensor_tensor(out=qdec[:, :], in0=p[:, :], scalar=128 ** -0.5, in1=eb[:, :], op0=ALU.mult, op1=ALU.mult),
                  r=[pb, ebb], w=[qdecb])
                for c in range(4):
                    dcol = dprev[:, 0:1] if c == 0 else eb[:, c * 128 - 1:c * 128]
                    V(lambda e: e.tensor_scalar(out=q2[:, c * 128:(c + 1) * 128], in0=qdec[:, c * 128:(c + 1) * 128], scalar1=dcol, scalar2=None, op0=ALU.mult),
                      r=[qdecb, dprevb, ebb], pw=[q2b])
                p, pb = inproj_fm(hs, hsb, C_GK, 128)
                V(lambda e: e.tensor_tensor(out=kinv[:, :], in0=p[:, :], in1=enb[:, :], op=ALU.mult), r=[pb, enbb], w=[kinvb])
            if 'mqk' in STAGES:
                for kind in ("k", "q"):
                    for h in range(2):
                        col = (C_MKA, C_MKB)[h] if kind == "k" else (C_MQA, C_MQB)[h]
                        gcol = 2 if kind == "k" else 1
                        p, pb = inproj_fm(hs, hsb, col, 64)
                        LV = MQK_LEVEL[0]
                        if LV >= 2: A(lambda e: e.activation(out=sq[:, :], in_=p[0:64, :], func=AF.Square), r=[pb], w=[sqb])
                        pss, pssb = nextps()
                        if LV >= 3: MM(lambda e: e.matmul(pss[0:64, :], lhsT=ones_b[0:64, 0:64], rhs=sq[:, :], start=True, stop=True), r=[onb, sqb], w=[pssb])
                        if LV >= 4: A(lambda e: e.activation(out=rs[:, :], in_=pss[0:64, :], func=AF.Ln, scale=1.0 / 64, bias=epsc[0:64, 0:1]), r=[pssb, epsb], w=[rsb])
                        if LV >= 4: A(lambda e: e.activation(out=rs[:, :], in_=rs[:, :], func=AF.Exp, scale=-0.5), r=[rsb], w=[rsb])
                        if LV >= 5: A(lambda e: e.activation(out=kg[:, :], in_=p[0:64, :], func=AF.Identity, scale=pp_sb[0:64, gcol:gcol + 1]),
                          r=[pb, ppb], w=[kgb])
                        pr, prb = nextps()
                        if LV >= 6: MM(lambda e: e.matmul(pr[0:64, :], lhsT=RT, rhs=kg[:, :], start=True, stop=True), r=[cbb, kgb], w=[prb])
                        if LV >= 7: V(lambda e: e.tensor_tensor(out=ta[:, :], in0=kg[:, :], in1=cs_sb[:, 0, :], op=ALU.mult), r=[kgb, csb], w=[tab])
                        if LV >= 8: V(lambda e: e.tensor_tensor(out=tb[:, :], in0=pr[0:64, :], in1=cs_sb[:, 1, :], op=ALU.mult), r=[prb, csb], w=[tbb])
                        if LV >= 9: V(lambda e: e.tensor_tensor(out=ta[:, :], in0=ta[:, :], in1=tb[:, :], op=ALU.add), r=[tab, tbb], w=[tab])
                        if kind == "k":
                            if LV >= 10: V(lambda e: e.tensor_tensor(out=kaug[h][0:64, ts:ts + 512], in0=ta[:, :], in1=rs[:, :], op=ALU.mult),
                              r=[tab, rsb], pw=[kaugb[h]])
                            if LV >= 11: V(lambda e: e.tensor_reduce(out=km[:, :], in_=kaug[h][0:64, ts:ts + 512].rearrange("p (b k) -> p b k", k=256), axis=AX.X, op=ALU.add),
                              r=[kaugb[h]], w=[kmb])
                            if LV >= 11: V(lambda e: e.tensor_scalar(out=KmT[:, h, 2 * t:2 * t + 2], in0=km[:, :], scalar1=1.0 / 256, scalar2=None, op0=ALU.mult),
                              r=[kmb], pw=[KmTb])
                        else:
                            if LV >= 10: V(lambda e: e.scalar_tensor_tensor(out=qaug[h][0:64, :], in0=ta[:, :], scalar=0.125, in1=rs[:, :], op0=ALU.mult, op1=ALU.mult),
                              r=[tab, rsb], pw=[qaugb[h]])
                            for s in range(4 if ('gate' in STAGES and LV >= 12) else 0):
                                ob = (ts + s * 128) // 256
                                pgt, pgtb = nextps()
                                MM(lambda e: e.matmul(pgt[:, 0:32], lhsT=qaug[h][0:64, s * 128:(s + 1) * 128], rhs=KmT[:, h, :], start=True, stop=True),
                                   r=[qaugb[h], KmTb], w=[pgtb])
                                V(lambda e: e.memset(gsb[:, :], -1e30), w=[gsbb])
                                if ob > 0:
                                    V(lambda e: e.tensor_copy(out=gsb[:, 0:ob], in_=pgt[:, 0:ob]), r=[pgtb], w=[gsbb])
                                V(lambda e: e.max(out=mx8[:, :], in_=gsb[:, :]), r=[gsbb], w=[mx8b])
                                V(lambda e: e.tensor_tensor(out=gsb[:, :], in0=gsb[:, :], in1=mx8[:, 2:3].to_broadcast([128, 32]), op=ALU.is_lt),
                                  r=[gsbb, mx8b], w=[gsbb])
                                V(lambda e: e.tensor_scalar(out=mb96[:, 64:96], in0=gsb[:, :], scalar1=NEG, scalar2=None, op0=ALU.mult),
                                  r=[gsbb], w=[mb96b])
                                V(lambda e: e.memset(mb96[:, 64 + ob:65 + ob], 0.0), w=[mb96b])
                                MM(lambda e: e.transpose(out=pst[0:96, 0:128], in_=mb96[:, :], identity=ident_b), r=[mb96b, cbb], w=[pstb])
                                A(lambda e: e.copy(out=qaug[h][64:96, s * 128:(s + 1) * 128], in_=pst[64:96, 0:128]), r=[pstb], pw=[qaugb[h]])
            if 'mlc' in STAGES:
                for i, (col, dst, dstb) in enumerate(((C_LQ, qc, qcb), (C_LK, kc, kcb))):
                    p, pb = inproj_fm(hs, hsb, col, 64)
                    A(lambda e: e.copy(out=xq[i][:, 3:515], in_=p[0:64, :]), r=[pb], w=[xqb[i]])
                    c0 = 3 + 4 * i
                    A(lambda e: e.activation(out=cva[:, :], in_=xq[i][:, 0:512], func=AF.Identity, scale=ppc[0:64, c0:c0 + 1]), r=[xqb[i], ppcb], w=[cvab])
                    for wi in (1, 2, 3):
                        A(lambda e: e.activation(out=ta[:, :], in_=xq[i][:, wi:wi + 512], func=AF.Identity, scale=ppc[0:64, c0 + wi:c0 + wi + 1]), r=[xqb[i], ppcb], w=[tab])
                        if wi < 3:
                            V(lambda e: e.tensor_tensor(out=cva[:, :], in0=cva[:, :], in1=ta[:, :], op=ALU.add), r=[cvab, tab], w=[cvab])
                        else:
                            V(lambda e: e.tensor_tensor(out=dst[:, :], in0=cva[:, :], in1=ta[:, :], op=ALU.add), r=[cvab, tab], w=[dstb])
                    V(lambda e: e.tensor_copy(out=xq[i][:, 0:3], in_=xq[i][:, 512:515]), r=[xqb[i]], w=[xqb[i]])
            if 'tm' in STAGES:
                for s in range(4):
                    kt = t * 4 + s
                    for (pbank, pbk, c0, n) in ((PS[2], PSb[2], C_TM1, 384), (PS[3], PSb[3], C_TM2, 258)):
                        for c in range(8):
                            MM(lambda e: e.matmul(pbank[:, 0:n], lhsT=hs[:, c, s * 128:(s + 1) * 128], rhs=w_sb[:, c, c0:c0 + n], start=(c == 0), stop=(c == 7)),
                               r=[wb, hsb], w=[pbk], inc=(c == 7))
                    p2_, p3_ = PS[2], PS[3]
                    if TM_LEVEL[0] >= 2: A(lambda e: e.copy(out=vtm[:, s, :], in_=p2_[:, 0:384]), r=[PSb[2]], pw=[vtmb])
                    if TM_LEVEL[0] >= 3: V(lambda e: e.tensor_copy(out=Vst[:, kt, :, 0:64], in_=vtm[:, s, 128:256].rearrange("p (h d) -> p h d", d=64)), r=[vtmb], pw=[Vstb])
                    if TM_LEVEL[0] >= 4: V(lambda e: e.tensor_copy(out=vml[:, s, 0:128], in_=vtm[:, s, 256:384]), r=[vtmb], pw=[vmlb])
                    if TM_LEVEL[0] >= 5: A(lambda e: e.activation(out=ee[:, :], in_=p3_[:, 0:256], func=AF.Exp, scale=-1.0), r=[PSb[3]], w=[eeb])
                    if TM_LEVEL[0] >= 5: V(lambda e: e.tensor_scalar(out=ee[:, :], in0=ee[:, :], scalar1=1.0, scalar2=None, op0=ALU.add), r=[eeb], w=[eeb])
                    if TM_LEVEL[0] >= 5: V(lambda e: e.reciprocal(out=ee[:, :], in_=ee[:, :]), r=[eeb], w=[eeb])
                    if TM_LEVEL[0] >= 6: V(lambda e: e.tensor_tensor(out=gmo[:, s, :], in0=ee[:, 0:128], in1=bc_sb[:, 128:256], op=ALU.mult), r=[eeb, bcb], pw=[gmob])
                    if TM_LEVEL[0] >= 7: V(lambda e: e.tensor_tensor(out=gmg[:, s, :], in0=ee[:, 128:256], in1=p3_[:, 128:256], op=ALU.mult), r=[eeb, PSb[3]], pw=[gmgb])
                    if TM_LEVEL[0] >= 8: G(lambda e: e.tensor_tensor(out=gmg[:, s, :], in0=gmg[:, s, :], in1=bc_sb[:, 0:128], op=ALU.mult), r=[gmgb, bcb], pw=[gmgb])
                    if TM_LEVEL[0] >= 9: V(lambda e: e.tensor_tensor(out=li[:, s:s + 1], in0=p3_[:, 256:257], in1=bc_sb[:, 256:257], op=ALU.add), r=[PSb[3], bcb], pw=[lib])
                    if TM_LEVEL[0] >= 10: A(lambda e: e.activation(out=nlf[:, s:s + 1], in_=p3_[:, 257:258], func=AF.Exp, scale=-1.0, bias=neg_sb[:, 1:2]), r=[PSb[3], negb_], pw=[nlfb])
                    if TM_LEVEL[0] >= 10: A(lambda e: e.activation(out=nlf[:, s:s + 1], in_=nlf[:, s:s + 1], func=AF.Ln, bias=1.0), r=[nlfb], pw=[nlfb])
            if 'gla' in STAGES:
                for c in range(4):
                    cs_ = slice(c * 128, (c + 1) * 128)
                    n = t * 4 + c
                    Wold, Woldb = Wst[n % 2], Wstb[n % 2]
                    Wnew, Wnewb = Wst[(n + 1) % 2], Wstb[(n + 1) % 2]
                    dcol = dprev[:, 0:1] if c == 0 else eb[:, c * 128 - 1:c * 128]
                    if GL[0] >= 1: MM(lambda e: e.transpose(out=pst[:, 128:256], in_=kinv[:, cs_], identity=ident_b), r=[kinvb, cbb], w=[pstb])
                    if GL[0] >= 2: A(lambda e: e.copy(out=kinvT[:, :], in_=pst[:, 128:256]), r=[pstb], w=[kinvTb])
                    pa, pab = nextps()
                    if GL[0] >= 3: MM(lambda e: e.matmul(pa[:, 0:128], lhsT=kinv[:, cs_], rhs=qdec[:, cs_], start=True, stop=True), r=[kinvb, qdecb], w=[pab])
                    if GL[0] >= 4: V(lambda e: e.tensor_tensor(out=attn[:, :], in0=pa[:, 0:128], in1=mask_f, op=ALU.mult), r=[pab, cfb], w=[attnb])
                    po, pob = nextps()
                    if GL[0] >= 5: MM(lambda e: e.matmul(po[:, 0:128], lhsT=attn[:, :], rhs=vtm[:, c, 0:128], start=True, stop=False), r=[attnb, vtmb], w=[pob], inc=False)
                    if GL[0] >= 6: MM(lambda e: e.matmul(po[:, 0:128], lhsT=q2[:, cs_], rhs=Wold[:, :], start=False, stop=True), r=[q2b, Woldb], w=[pob])
                    if GL[0] >= 7: MM(lambda e: e.matmul(po[:, 128:256], lhsT=kinvT[:, :], rhs=vtm[:, c, 0:128], start=True, stop=True), r=[kinvTb, vtmb], w=[pob])
                    if GL[0] >= 8: A(lambda e: e.activation(out=DTm[:, :], in_=Wold[:, :], func=AF.Identity, scale=dcol), r=[Woldb, dprevb, ebb], w=[DTmb])
                    if GL[0] >= 9: V(lambda e: e.tensor_tensor(out=Wnew[:, :], in0=DTm[:, :], in1=po[:, 128:256], op=ALU.add), r=[DTmb, pob], w=[Wnewb])
                    if GL[0] >= 10: A(lambda e: e.copy(out=obg[:, c, :], in_=po[:, 0:128]), r=[pob], pw=[obgb])
                    if GL[0] >= 11: V(lambda e: e.tensor_tensor(out=ubg[:, c, :], in0=po[:, 0:128], in1=gmg[:, c, :], op=ALU.mult), r=[pob, gmgb], pw=[ubgb])
                if GL[0] >= 15: V(lambda e: e.tensor_copy(out=dprev[:, :], in_=eb[:, 511:512]), r=[ebb], w=[dprevb])
            if 'ml' in STAGES:
                for c in range(4):
                    cs_ = slice(c * 128, (c + 1) * 128)
                    A(lambda e: e.activation(out=Am[:, :], in_=SL_f, func=AF.Identity, scale=nlf[:, c:c + 1]), r=[cfb, nlfb], w=[Amb])
                    A(lambda e: e.activation(out=nbc[:, :], in_=ones_f[:, :], func=AF.Identity, scale=nlf[:, c:c + 1]), r=[onfb, nlfb], w=[nbcb])
                    pg, pgb = nextps()
                    MM(lambda e: e.matmul(pg[:, 0:128], lhsT=Am[:, :], rhs=mask_f, start=True, stop=True), r=[Amb, cfb], w=[pgb])
                    MM(lambda e: e.matmul(pg[:, 128:129], lhsT=mask_f, rhs=nlf[:, c:c + 1], start=True, stop=True), r=[cfb, nlfb], w=[pgb])
                    MM(lambda e: e.matmul(pg[0:64, 129:130], lhsT=nbc[:, :], rhs=ones_f[:, 0:1], start=True, stop=True), r=[nbcb, onfb], w=[pgb])
                    A(lambda e: e.activation(out=DT[:, :], in_=pg[:, 0:128], func=AF.Exp, scale=-1.0, bias=li[:, c:c + 1]), r=[pgb, lib], w=[DTb])
                    A(lambda e: e.activation(out=st1[:, 1:2], in_=pg[:, 128:129], func=AF.Exp, scale=-1.0), r=[pgb], pw=[st1b])
                    A(lambda e: e.activation(out=efl[:, :], in_=pg[0:64, 129:130], func=AF.Exp, scale=-1.0), r=[pgb], w=[eflb])
                    G(lambda e: e.tensor_tensor(out=DTm[:, :], in0=DT[:, :], in1=mask_f, op=ALU.mult), r=[DTb, cfb], w=[DTmb])
                    psx, psxb = nextps()
                    MM(lambda e: e.matmul(psx[:, 0:128], lhsT=kc[:, cs_], rhs=qc[:, cs_], start=True, stop=True), r=[kcb, qcb], w=[psxb])
                    V(lambda e: e.scalar_tensor_tensor(out=sT[:, :], in0=psx[:, 0:128], scalar=1.0, in1=DTm[:, :], op0=ALU.mult, op1=ALU.mult),
                      r=[psxb, DTmb], w=[sTb])
                    pn, pnb = nextps()
                    MM(lambda e: e.matmul(pn[:, 0:129], lhsT=sT[:, :], rhs=vml[:, c, :], start=True, stop=True), r=[sTb, vmlb], w=[pnb])
                    MM(lambda e: e.matmul(pn[:, 256:385], lhsT=qc[:, cs_], rhs=Cbf[:, :], start=True, stop=True), r=[qcb, Cbfb], w=[pnb])
                    A(lambda e: e.activation(out=tot[:, :], in_=pn[:, 256:385], func=AF.Identity, scale=st1[:, 1:2]), r=[pnb, st1b], w=[totb])
                    V(lambda e: e.tensor_tensor(out=tot[:, :], in0=tot[:, :], in1=pn[:, 0:129], op=ALU.add), r=[totb, pnb], w=[totb])
                    A(lambda e: e.activation(out=st1[:, 2:3], in_=tot[:, 128:129], func=AF.Abs), r=[totb], pw=[st1b])
                    V(lambda e: e.tensor_scalar(out=st1[:, 2:3], in0=st1[:, 2:3], scalar1=1.0, scalar2=None, op0=ALU.max), r=[st1b], pw=[st1b])
                    V(lambda e: e.reciprocal(out=st1[:, 2:3], in_=st1[:, 2:3]), r=[st1b], pw=[st1b])
                    A(lambda e: e.activation(out=hh[:, :], in_=tot[:, 0:128], func=AF.Identity, scale=st1[:, 2:3]), r=[totb, st1b], w=[hhb])
                    A(lambda e: e.copy(out=obm[:, c, :], in_=hh[:, :]), r=[hhb], pw=[obmb])
                    V(lambda e: e.tensor_tensor(out=ubm[:, c, :], in0=hh[:, :], in1=gmo[:, c, :], op=ALU.mult), r=[hhb, gmob], pw=[ubmb])
                    MM(lambda e: e.transpose(out=pst[:, 256:320], in_=kc[:, cs_], identity=ident_b[0:64, 0:64]), r=[kcb, cbb], w=[pstb])
                    A(lambda e: e.activation(out=kw[:, :], in_=pst[:, 256:320], func=AF.Identity, scale=DT[:, 127:128]), r=[pstb, DTb], w=[kwb])
                    pk, pkb = nextps()
                    MM(lambda e: e.matmul(pk[0:64, 0:129], lhsT=kw[:, :], rhs=vml[:, c, :], start=True, stop=True), r=[kwb, vmlb], w=[pkb])
                    A(lambda e: e.activation(out=C32[:, :], in_=C32[:, :], func=AF.Identity, scale=efl[:, 0:1]), r=[C32b, eflb], w=[C32b])
                    V(lambda e: e.tensor_tensor(out=C32[:, :], in0=C32[:, :], in1=pk[0:64, 0:129], op=ALU.add), r=[C32b, pkb], w=[C32b])
                    A(lambda e: e.copy(out=Cbf[:, :], in_=C32[:, :]), r=[C32b], w=[Cbfb])
            if 'moba' in STAGES:
                for h in range(2):
                    acc, accb = PS[2 + h], PSb[2 + h]
                    nk = (t + 1) * 4
                    for kt in range(nk):
                        r_ = kt - t * 4
                        q0 = 0 if r_ < 0 else r_ * 128
                        psS, psSb = nextps()
                        diag = r_ >= 0
                        MM(lambda e: e.matmul(psS[:, q0:512], lhsT=kaug[h][0:96, kt * 128:(kt + 1) * 128], rhs=qaug[h][0:96, q0:512], start=True, stop=not diag),
                           r=[kaugb[h], qaugb[h]], w=[psSb], inc=not diag)
                        if diag:
                            MM(lambda e: e.matmul(psS[:, q0:512], lhsT=ident_b, rhs=CBd[:, 0:512 - q0], start=False, stop=True), r=[cbb], w=[psSb])
                        P_, P_b = Pb[kt % 2], Pbb[kt % 2]
                        A(lambda e: e.activation(out=P_[:, q0:512], in_=psS[:, q0:512], func=AF.Exp), r=[psSb], w=[P_b])
                        for s in range(max(r_, 0), 4):
                            MM(lambda e: e.matmul(acc[:, s * 65:(s + 1) * 65], lhsT=P_[:, s * 128:(s + 1) * 128], rhs=Vst[:, kt, h, :],
                                                  start=(kt == 0 and s == 0), stop=(kt == nk - 1), skip_group_check=True),
                               r=[P_b, Vstb], w=[accb], inc=(s == 3))
                    A(lambda e: e.copy(out=accs[:, :], in_=acc[:, 0:260]), r=[accb], w=[accsb])
                    accv = accs[:, 0:260].rearrange("p (s d) -> p s d", d=65)
                    V(lambda e: e.reciprocal(out=rec4[:, :], in_=accv[:, :, 64]), r=[accsb], w=[rec4b])
                    for s in range(4):
                        A(lambda e: e.activation(out=ytok[:, s, 128 + h * 64:192 + h * 64], in_=accs[:, s * 65:s * 65 + 64], func=AF.Identity, scale=rec4[:, s:s + 1]),
                          r=[accsb, rec4b], pw=[ytokb])
            if 'out' in STAGES:
                for s in range(4):
                    scol = slice(s * 128, (s + 1) * 128)
                    srcs = ((obg[:, s, :], obgb, 512), (ubg[:, s, :], ubgb, 640), (ytok[:, s, 128:256], ytokb, 768), (obm[:, s, :], obmb, 896), (ubm[:, s, :], ubmb, 384))
                    for k_, (src, srcb, off) in enumerate(srcs):
                        MM(lambda e: e.transpose(out=pst[:, off:off + 128], in_=src, identity=ident_b), r=[srcb, cbb], w=[pstb], inc=(k_ == 4))
                    for br, ooff, uoff in ((0, 512, 640), (2, 896, 384)):
                        A(lambda e: e.activation(out=osq[:, :], in_=pst[:, ooff:ooff + 128], func=AF.Square), r=[pstb], w=[osqb])
                        pss, pssb = nextps()
                        MM(lambda e: e.matmul(pss[:, 0:128], lhsT=ones_b[:, :], rhs=osq[:, :], start=True, stop=True), r=[onb, osqb], w=[pssb])
                        A(lambda e: e.activation(out=rsn[:, :], in_=pss[:, 0:128], func=AF.Ln, scale=1.0 / 128, bias=epsc[:, 0:1]), r=[pssb, epsb], w=[rsnb])
                        A(lambda e: e.activation(out=rsn[:, :], in_=rsn[:, :], func=AF.Exp, scale=-0.5), r=[rsnb], w=[rsnb])
                        V(lambda e: e.tensor_tensor(out=yT_sb[:, br, scol], in0=pst[:, uoff:uoff + 128], in1=rsn[:, :], op=ALU.mult), r=[pstb, rsnb], pw=[yTb])
                    A(lambda e: e.copy(out=yT_sb[:, 1, scol], in_=pst[:, 768:896]), r=[pstb], pw=[yTb])
                S.dma("sync", yTr[:, :, ts:ts + 512], yT_sb[:, :, :], reads=[yTb], pwrites=[yTdb], sem_owner=yTb)
        S.finish([yTdb])
    return nc


NEG = -30000.0
GLA_K = 512; MOBA_W = 512; MQK = 256; MV = 512
OFF = {}
_splits = [("g_q",512),("g_k",512),("g_v",512),("g_g",512),("g_a",16),("m_q",512),("m_k",512),("m_v",512),
           ("l_q",256),("l_k",256),("l_v",512),("l_o",512),("l_i",4),("l_f",4),("br",3072)]
_o = 0
for n, s in _splits:
    OFF[n] = _o; _o += s

def p2_cols(g):
    r = lambda name, a, b: list(range(OFF[name] + a, OFF[name] + b))
    cols = []
    cols += r("g_q", g*128, g*128+128) + r("g_k", g*128, g*128+128) + r("g_a", 0, 16)
    cols += r("m_q", g*128, g*128+64) + r("m_q", g*128+64, g*128+128) + r("m_k", g*128, g*128+64) + r("m_k", g*128+64, g*128+128)
    cols += r("l_q", g*64, g*64+64) + r("l_k", g*64, g*64+64)
    cols += r("g_v", g*128, g*128+128) + r("m_v", g*128, g*128+128) + r("l_v", g*128, g*128+128)
    cols += r("l_o", g*128, g*128+128) + r("g_g", g*128, g*128+128) + r("l_i", g, g+1) + r("l_f", g, g+1)
    assert len(cols) == 1298
    return np.array(cols)

def consts(T):
    c = np.zeros((128, 1472), np.float32)
    j = np.arange(128)[:, None]; i = np.arange(128)[None, :]
    c[:, 0:128] = (j <= i)
    c[:, 128:256] = (j > i)
    c[:, 256:384] = np.eye(128)
    rs = np.ones((128, 512), np.float32); rs[:, [0, 128, 256, 384]] = 0
    c[:, 384:896] = rs
    ii = np.arange(512)[None, :]
    c[:, 896:1408] = np.where(j <= ii, 0.0, NEG)
    RT = np.zeros((64, 64), np.float32)
    for m in range(32):
        RT[m + 32, m] = -1.0
    for m in range(32, 64):
        RT[m - 32, m] = 1.0
    c[0:64, 1408:1472] = RT
    inv = (1.0 / (np.float32(10000.0) ** (np.arange(0, 64, 2, dtype=np.float32) / np.float32(64)))).astype(np.float32)
    ang = np.arange(T, dtype=np.float32)[:, None] * inv[None, :]
    cos = np.cos(ang).astype(np.float32).T; sin = np.sin(ang).astype(np.float32).T
    cosT = np.ascontiguousarray(np.concatenate([cos, cos], 0)); sinT = np.ascontiguousarray(np.concatenate([sin, sin], 0))
    onehot = (np.arange(T)[None, :] // 256 == np.arange(32)[:, None]).astype(np.float32)
    return dict(cst=c, cosT=cosT, sinT=sinT, onehot=onehot)

def prep_p2(I, l, g):
    w_in = I["w_in"][l]
    wsel = w_in[:, p2_cols(g)]
    w = np.ascontiguousarray(wsel.reshape(8, 128, 1298).transpose(1, 0, 2))
    pp = np.zeros((128, 16), np.float32)
    pp[:, 0] = I["gla_a_b"][l][g*128:(g+1)*128]
    pp[0:64, 1] = I["moba_qn_g"][l]; pp[0:64, 2] = I["moba_kn_g"][l]
    cw = I["mlstm_conv_w"][l]
    pp[0:64, 3:7] = cw[:, g*64:(g+1)*64].T
    pp[0:64, 7:11] = cw[:, 256 + g*64:256 + (g+1)*64].T
    bc = np.zeros((128, 258), np.float32)
    bc[:, 0:128] = I["gla_norm_g"][l][None, :]; bc[:, 128:256] = I["mlstm_norm_g"][l][None, :]
    bc[:, 256] = I["mlstm_i_b"][l][g]; bc[:, 257] = I["mlstm_f_b"][l][g]
    up = np.ascontiguousarray(I["gla_a_up"][l][:, g*128:(g+1)*128])
    return dict(w=w, pp=pp, bc=bc, up=up)


from concourse.bass_utils import run_bass_kernel_spmd
import ml_dtypes

_PROG = {}


def _prog(key, fn):
    if key not in _PROG:
        _PROG[key] = fn()
    return _PROG[key]


def _tiles(w, kc):
    K, N = w.shape
    return np.ascontiguousarray(w.reshape(kc, 128, N // 128, 128).transpose(2, 1, 0, 3))


def _pp(v):
    return np.ascontiguousarray(v.reshape(-1, 128).T)


def kernel(**inputs):
    I = {k: np.asarray(v) for k, v in inputs.items()}
    x = I["x"].astype(np.float32)
    B, T, Dm = x.shape
    L = I["w_in"].shape[0]
    NTOK = T // 4
    cores = list(range(8))
    xT = [np.ascontiguousarray(x[c // 4, (c % 4) * NTOK:(c % 4 + 1) * NTOK, :].T) for c in cores]
    nc0 = _prog("p1", lambda: build_p3(NTOK, False, first_only=True))
    res = run_bass_kernel_spmd(nc0, [dict(xT=xT[c], g1n=_pp(I["norm1_g"][0])) for c in cores], core_ids=cores).results
    hn = [np.asarray(res[c]["hno"]) for c in cores]
    cst = consts(T)
    nc2 = _prog("p2", lambda: build_p2(T))
    for l in range(L):
        last = (l == L - 1)
        in2 = []
        for c in cores:
            b, g = c // 4, c % 4
            d = prep_p2(I, l, g)
            d.update(cst)
            d["hnT"] = np.ascontiguousarray(np.concatenate([hn[b * 4 + j] for j in range(4)], axis=1))
            in2.append(d)
        res2 = run_bass_kernel_spmd(nc2, in2, core_ids=cores).results
        yT = [np.asarray(res2[c]["yT"]) for c in cores]
        wl = dict(wg=_tiles(np.ascontiguousarray(I["w_in"][l][:, OFF["br"]:]), 8), gbias=_pp(I["gate_b"][l]),
                  wbr=np.concatenate([_tiles(I["w_br_gla"][l], 4), _tiles(I["w_br_moba"][l], 4), _tiles(I["w_br_mlstm"][l], 4)], 0),
                  wo=_tiles(I["w_out"][l], 8), g2=_pp(I["norm2_g"][l]), w1=_tiles(I["w_ff1"][l], 8), w2=_tiles(I["w_ff2"][l], 32),
                  g1n=_pp(I["norm1_g"][min(l + 1, L - 1)]))
        nc3 = _prog("p3_%d" % last, lambda: build_p3(NTOK, last))
        in3 = []
        for c in cores:
            b, j = c // 4, c % 4
            d = dict(wl)
            d["xT"] = xT[c]
            d["hnT"] = hn[c]
            d["yT"] = np.ascontiguousarray(np.concatenate([yT[b * 4 + g][:, j * NTOK:(j + 1) * NTOK] for g in range(4)], axis=0))
            in3.append(d)
        res3 = run_bass_kernel_spmd(nc3, in3, core_ids=cores).results
        xT = [np.asarray(res3[c]["xo"]) for c in cores]
        if not last:
            hn = [np.asarray(res3[c]["hno"]) for c in cores]
    out = np.empty((B, T, Dm), np.float32)
    for c in cores:
        out[c // 4, (c % 4) * NTOK:(c % 4 + 1) * NTOK, :] = xT[c].T
    return out
```

```python
import numpy as np
import concourse.bass as bass
import concourse.mybir as mybir
from contextlib import ExitStack

F32 = mybir.dt.float32
BF16 = mybir.dt.bfloat16
AF = mybir.ActivationFunctionType
ALU = mybir.AluOpType
AX = mybir.AxisListType


class Buf:
    __slots__ = ("ap", "w", "r", "dsem", "dcnt", "name", "excl")

    def __init__(self, ap, name=""):
        self.ap = ap
        self.w = {}
        self.r = {}
        self.dsem = None
        self.dcnt = 0
        self.name = name
        self.excl = False

    def __getitem__(self, idx):
        return self.ap[idx]


class EngW:
    def __init__(self, S, name):
        self.S = S
        self.name = name
        self.e = getattr(S.nc, name)
        self.sem = S.stack.enter_context(S.nc.semaphore("es_" + name))
        self.cnt = 0
        self.waited = {}

    def wait(self, ev):
        if ev is None:
            return
        sem, val = ev
        k = id(sem)
        if self.waited.get(k, 0) >= val:
            return
        self.e.wait_ge(sem, val)
        self.waited[k] = val


class Sched:
    def __init__(self, nc, stack):
        self.nc = nc
        self.stack = stack
        self.E = {n: EngW(self, n) for n in ("tensor", "vector", "scalar", "gpsimd", "sync")}
        self.nsem = 5
        self.pending = {}

    def sbuf(self, name, shape, dt):
        t = self.stack.enter_context(self.nc.sbuf_tensor(name, list(shape), dt))
        return t

    def psum(self, name, shape, dt=F32):
        t = self.stack.enter_context(self.nc.psum_tensor(name, list(shape), dt))
        return t

    def buf(self, ap, name=""):
        return Buf(ap, name)

    def newsem(self, name):
        self.nsem += 1
        return self.stack.enter_context(self.nc.semaphore(name))

    def _deps(self, E, reads, writes, pwrites, same_ok, skip_sem=None):
        for b in reads:
            for ev in b.w.values():
                E.wait(ev)
            if b.excl:
                for ev in b.r.values():
                    if ev[0] is not E.sem:
                        E.wait(ev)
        for b in writes:
            for ev in b.w.values():
                if not (same_ok and ev[0] is E.sem):
                    E.wait(ev)
            for ev in b.r.values():
                if not (same_ok and ev[0] is E.sem):
                    E.wait(ev)
        for b in pwrites:
            for ev in b.w.values():
                if ev[0] is not E.sem and ev[0] is not skip_sem:
                    E.wait(ev)
            for ev in b.r.values():
                if not (same_ok and ev[0] is E.sem):
                    E.wait(ev)

    def _record(self, ev, reads, writes, pwrites):
        k = id(ev[0])
        for b in reads:
            b.r[k] = ev
        for b in writes:
            b.w = {k: ev}
            b.r = {}
        for b in pwrites:
            b.w[k] = ev

    def op(self, eng, fn, reads=(), writes=(), pwrites=(), inc=True):
        E = self.E[eng]
        self._deps(E, reads, writes, pwrites, eng == "tensor")
        ins = fn(E.e)
        if inc:
            E.cnt += 1
            ins.then_inc(E.sem, 1)
            ev = (E.sem, E.cnt)
        else:
            ev = (E.sem, E.cnt + 1)
        self._record(ev, reads, writes, pwrites)
        return ev

    def dma(self, q, out_ap, in_ap, reads=(), writes=(), pwrites=(), sem_owner=None, **kw):
        E = self.E[q]
        owner = sem_owner or (writes[0] if writes else (pwrites[0] if pwrites else reads[0]))
        if owner.dsem is None:
            owner.dsem = self.newsem("ds_%d" % self.nsem)
        self._deps(E, reads, writes, pwrites, False, skip_sem=owner.dsem)
        ins = E.e.dma_start(out=out_ap, in_=in_ap, **kw)
        owner.dcnt += 16
        ins.then_inc(owner.dsem, 16)
        ev = (owner.dsem, owner.dcnt)
        self._record(ev, reads, writes, pwrites)
        return ev

    def finish(self, bufs, eng="sync"):
        E = self.E[eng]
        for b in bufs:
            for ev in b.w.values():
                E.wait(ev)
            for ev in b.r.values():
                E.wait(ev)


EPS = 1e-6


def emit_norm(S, K, x_sb, xb, g_sb, gb, out_sb, outb, ntok, ps_ss, ps_ssb, sq_sb, sqb, tmp_sb, tmpb):
    nc = S.nc
    for h in range(ntok // 512):
        hs = slice(h * 512, (h + 1) * 512)
        S.op("scalar", lambda e: e.activation(out=sq_sb[:, :, :], in_=x_sb[:, :, hs], func=AF.Square),
             reads=[xb], writes=[sqb])
        for c in range(8):
            S.op("tensor", lambda e: e.matmul(ps_ss[:, :], lhsT=K["ones"][:, :], rhs=sq_sb[:, c, :],
                                              start=(c == 0), stop=(c == 7)),
                 reads=[K["onesb"], sqb], writes=[ps_ssb], inc=(c == 7))
        S.op("scalar", lambda e: e.activation(out=tmp_sb[:, :], in_=ps_ss[:, :], func=AF.Ln, scale=1.0 / 1024, bias=K["eps"][:, 0:1]),
             reads=[ps_ssb, K["epsb"]], writes=[tmpb])
        S.op("scalar", lambda e: e.activation(out=tmp_sb[:, :], in_=tmp_sb[:, :], func=AF.Exp, scale=-0.5),
             reads=[tmpb], writes=[tmpb])
        for c in range(8):
            S.op("vector", lambda e: e.scalar_tensor_tensor(out=out_sb[:, c, hs], in0=x_sb[:, c, hs], scalar=g_sb[:, c:c + 1],
                                                            in1=tmp_sb[:, :], op0=ALU.mult, op1=ALU.mult),
                 reads=[xb, gb, tmpb], pwrites=[outb])


def build_p3(ntok, last, first_only=False, stop_after='Z'):
    nc = bass.Bass("TRN2", target_bir_lowering=False)
    D = lambda name, shape, dt=F32, kind="ExternalInput": nc.dram_tensor(name, list(shape), dt, kind=kind).ap()
    xT = D("xT", [1024, ntok])
    g1n = D("g1n", [128, 8])
    if not first_only:
        hnT = D("hnT", [1024, ntok], BF16)
        yT = D("yT", [1536, ntok], BF16)
        wg = D("wg", [24, 128, 8, 128]); gbias = D("gbias", [128, 24])
        wbr = D("wbr", [24, 128, 4, 128])
        wo = D("wo", [8, 128, 8, 128])
        g2 = D("g2", [128, 8])
        w1 = D("w1", [32, 128, 8, 128]); w2 = D("w2", [8, 128, 32, 128])
        xo = D("xo", [1024, ntok], F32, "ExternalOutput")
    if not last:
        hno = D("hno", [1024, ntok], BF16, "ExternalOutput")
    G = 1024
    with ExitStack() as st:
        S = Sched(nc, st)
        K = {}
        K["ones"] = S.sbuf("ones", [128, 128], BF16); K["onesb"] = S.buf(K["ones"])
        K["eps"] = S.sbuf("epsc", [128, 1], F32); K["epsb"] = S.buf(K["eps"])
        S.op("vector", lambda e: e.memset(K["ones"][:, :], 1.0), writes=[K["onesb"]])
        S.op("vector", lambda e: e.memset(K["eps"][:, :], EPS), writes=[K["epsb"]])
        x_sb = S.sbuf("x_sb", [128, 8, G], F32); xb = S.buf(x_sb)
        hn_sb = S.sbuf("hn_sb", [128, 8, G], BF16); hnb = S.buf(hn_sb)
        g1_sb = S.sbuf("g1_sb", [128, 8], F32); g1b = S.buf(g1_sb)
        sq_sb = S.sbuf("sq_sb", [128, 8, 512], BF16); sqb = S.buf(sq_sb)
        tmp_sb = S.sbuf("tmp_sb", [128, 512], F32); tmpb = S.buf(tmp_sb)
        PS = [S.psum("ps%d" % i, [128, 512]) for i in range(8)]
        PSb = [S.buf(p) for p in PS]
        S.dma("sync", g1_sb[:, :], g1n, writes=[g1b])
        xTr = xT.rearrange("(c p) t -> p c t", p=128)
        if not last:
            hnor = hno.rearrange("(c p) t -> p c t", p=128)
            hnob = S.buf(hno)
        if first_only:
            for gi in range(ntok // G):
                gs = slice(gi * G, (gi + 1) * G)
                S.dma("sync", x_sb[:, :, :], xTr[:, :, gs], writes=[xb])
                emit_norm(S, K, x_sb, xb, g1_sb, g1b, hn_sb, hnb, G, PS[0], PSb[0], sq_sb, sqb, tmp_sb, tmpb)
                S.dma("sync", hnor[:, :, gs], hn_sb[:, :, :], reads=[hnb], pwrites=[hnob], sem_owner=hnb)
            S.finish([hnob])
            return nc
        mixed = S.sbuf("mixed", [128, 8, G], BF16); mixb = S.buf(mixed)
        big = S.sbuf("big", [128, 32, G], BF16); bigb = S.buf(big)
        NW = 3
        wt = [S.sbuf("wt%d" % i, [128, 32 * 128], BF16) for i in range(NW)]
        wtb = [S.buf(w) for w in wt]
        wctr = [0]
        gb_sb = S.sbuf("gb_sb", [128, 24], F32); gbb = S.buf(gb_sb)
        g2_sb = S.sbuf("g2_sb", [128, 8], F32); g2b = S.buf(g2_sb)
        acc = S.sbuf("acc", [128, G], F32); accb = S.buf(acc)
        t1 = [S.sbuf("t1_%d" % i, [128, 512], F32) for i in range(2)]; t1b = [S.buf(t) for t in t1]
        S.dma("sync", gb_sb[:, :], gbias, writes=[gbb])
        S.dma("sync", g2_sb[:, :], g2, writes=[g2b])
        S.op("vector", lambda e: e.tensor_scalar(out=gb_sb[:, :], in0=gb_sb[:, :], scalar1=-1.0, scalar2=None, op0=ALU.mult),
             reads=[gbb], writes=[gbb])
        xob = S.buf(xo)
        xor_ = xo.rearrange("(c p) t -> p c t", p=128)
        hnTr = hnT.rearrange("(c p) t -> p c t", p=128)
        yTr = yT.rearrange("(c p) t -> p c t", p=128)

        def wtile(src, kc):
            i = wctr[0] % NW
            wctr[0] += 1
            view = wt[i][:, 0:kc * 128].rearrange("p (k n) -> p k n", n=128)
            S.dma("gpsimd", view, src, writes=[wtb[i]])
            return view, wtb[i]

        psi = [0]

        def nextps():
            i = psi[0] % 6
            psi[0] += 1
            return PS[i], PSb[i]

        tci = [0]
        for gi in range(ntok // G):
            gs = slice(gi * G, (gi + 1) * G)
            S.dma("sync", x_sb[:, :, :], xTr[:, :, gs], writes=[xb])
            S.dma("sync", hn_sb[:, :, :], hnTr[:, :, gs], writes=[hnb])
            S.dma("sync", big[:, 0:12, :], yTr[:, :, gs], writes=[bigb])
            for m in range(8 if stop_after >= 'B' else 0):
                for br in range(3):
                    j = br * 8 + m
                    wgt, wgtb = wtile(wg[j], 8)
                    wbt, wbtb = wtile(wbr[j], 4)
                    for h in range(2):
                        hs = slice(h * 512, (h + 1) * 512)
                        pg, pgb = nextps()
                        for c in range(8):
                            S.op("tensor", lambda e: e.matmul(pg[:, :], lhsT=wgt[:, c, :], rhs=hn_sb[:, c, hs], start=(c == 0), stop=(c == 7)),
                                 reads=[wgtb, hnb], writes=[pgb], inc=(c == 7))
                        pb, pbb = nextps()
                        for g in range(4):
                            S.op("tensor", lambda e: e.matmul(pb[:, :], lhsT=wbt[:, g, :], rhs=big[:, g * 3 + br, hs], start=(g == 0), stop=(g == 3)),
                                 reads=[wbtb, bigb], writes=[pbb], inc=(g == 3))
                        ti = tci[0] % 2; tci[0] += 1
                        tt, ttb = t1[ti], t1b[ti]
                        S.op("scalar", lambda e: e.activation(out=tt[:, :], in_=pg[:, :], func=AF.Exp, scale=-1.0, bias=gb_sb[:, j:j + 1]),
                             reads=[pgb, gbb], writes=[ttb])
                        S.op("scalar", lambda e: e.activation(out=tt[:, :], in_=tt[:, :], func=AF.Ln, bias=1.0),
                             reads=[ttb], writes=[ttb])
                        S.op("scalar", lambda e: e.activation(out=tt[:, :], in_=tt[:, :], func=AF.Exp, scale=-1.0),
                             reads=[ttb], writes=[ttb])
                        if br == 0:
                            S.op("vector", lambda e: e.tensor_tensor(out=acc[:, hs], in0=tt[:, :], in1=pb[:, :], op=ALU.mult),
                                 reads=[ttb, pbb], pwrites=[accb])
                        else:
                            S.op("vector", lambda e: e.tensor_tensor(out=tt[:, :], in0=tt[:, :], in1=pb[:, :], op=ALU.mult),
                                 reads=[ttb, pbb], writes=[ttb])
                            if br == 1:
                                S.op("vector", lambda e: e.tensor_tensor(out=acc[:, hs], in0=acc[:, hs], in1=tt[:, :], op=ALU.add),
                                     reads=[ttb, accb], pwrites=[accb])
                            else:
                                S.op("vector", lambda e: e.tensor_tensor(out=mixed[:, m, hs], in0=acc[:, hs], in1=tt[:, :], op=ALU.add),
                                     reads=[ttb, accb], pwrites=[mixb])
            for m in range(8 if stop_after >= 'C' else 0):
                wot, wotb = wtile(wo[m], 8)
                for h in range(2):
                    hs = slice(h * 512, (h + 1) * 512)
                    p, pb_ = nextps()
                    for c in range(8):
                        S.op("tensor", lambda e: e.matmul(p[:, :], lhsT=wot[:, c, :], rhs=mixed[:, c, hs], start=(c == 0), stop=(c == 7)),
                             reads=[wotb, mixb], writes=[pb_], inc=(c == 7))
                    S.op("vector", lambda e: e.tensor_tensor(out=x_sb[:, m, hs], in0=x_sb[:, m, hs], in1=p[:, :], op=ALU.add),
                         reads=[pb_, xb], pwrites=[xb])
            emit_norm(S, K, x_sb, xb, g2_sb, g2b, hn_sb, hnb, G, PS[6], PSb[6], sq_sb, sqb, tmp_sb, tmpb)
            for f in range(32 if stop_after >= 'E' else 0):
                w1t, w1tb = wtile(w1[f], 8)
                for h in range(2):
                    hs = slice(h * 512, (h + 1) * 512)
                    p, pb_ = nextps()
                    for c in range(8):
                        S.op("tensor", lambda e: e.matmul(p[:, :], lhsT=w1t[:, c, :], rhs=hn_sb[:, c, hs], start=(c == 0), stop=(c == 7)),
                             reads=[w1tb, hnb], writes=[pb_], inc=(c == 7))
                    ti = tci[0] % 2; tci[0] += 1
                    tt, ttb = t1[ti], t1b[ti]
                    S.op("scalar", lambda e: e.activation(out=tt[:, :], in_=p[:, :], func=AF.Relu), reads=[pb_], writes=[ttb])
                    S.op("vector", lambda e: e.tensor_tensor(out=big[:, f, hs], in0=tt[:, :], in1=tt[:, :], op=ALU.mult),
                         reads=[ttb], pwrites=[bigb])
            for m in range(8 if stop_after >= 'F' else 0):
                w2t, w2tb = wtile(w2[m], 32)
                for h in range(2):
                    hs = slice(h * 512, (h + 1) * 512)
                    p, pb_ = nextps()
                    for f in range(32):
                        S.op("tensor", lambda e: e.matmul(p[:, :], lhsT=w2t[:, f, :], rhs=big[:, f, hs], start=(f == 0), stop=(f == 31)),
                             reads=[w2tb, bigb], writes=[pb_], inc=(f == 31))
                    S.op("vector", lambda e: e.tensor_tensor(out=x_sb[:, m, hs], in0=x_sb[:, m, hs], in1=p[:, :], op=ALU.add),
                         reads=[pb_, xb], pwrites=[xb])
            S.dma("sync", xor_[:, :, gs], x_sb[:, :, :], reads=[xb], pwrites=[xob], sem_owner=xb)
            if not last:
                emit_norm(S, K, x_sb, xb, g1_sb, g1b, hn_sb, hnb, G, PS[6], PSb[6], sq_sb, sqb, tmp_sb, tmpb)
                S.dma("sync", hnor[:, :, gs], hn_sb[:, :, :], reads=[hnb], pwrites=[hnob], sem_owner=hnb)
        S.finish([xob] + ([hnob] if not last else []))
    return nc


EPS = 1e-6
NEG = -30000.0
NCOL = 1298
MQK_LEVEL = [99]
GL = [99]
TM_LEVEL = [99]
STAGES = ['gd', 'mqk', 'gate', 'mlc', 'tm', 'gla', 'ml', 'moba', 'out']
C_GQ, C_GK, C_GA, C_MQA, C_MQB, C_MKA, C_MKB, C_LQ, C_LK = 0, 128, 256, 272, 336, 400, 464, 528, 592
C_TM1, C_TM2 = 656, 1040


def build_p2(T):
    nc = bass.Bass("TRN2", target_bir_lowering=False)
    D = lambda name, shape, dt=F32, kind="ExternalInput": nc.dram_tensor(name, list(shape), dt, kind=kind).ap()
    NT = T // 512
    NKT = T // 128
    NB = T // 256
    hnT = D("hnT", [1024, T], BF16)
    w = D("w", [128, 8, NCOL])
    pp = D("pp", [128, 16])
    bc = D("bc", [128, 258])
    up = D("up", [16, 128])
    cosT = D("cosT", [64, T]); sinT = D("sinT", [64, T])
    cst = D("cst", [128, 1472])
    onehot = D("onehot", [32, T])
    yT = D("yT", [384, T], BF16, "ExternalOutput")
    with ExitStack() as st:
        S = Sched(nc, st)
        sb = lambda name, shape, dt=F32: S.sbuf(name, shape, dt)
        cf = sb("cf", [128, 1472]); cfb = S.buf(cf)
        S.dma("sync", cf[:, :], cst[:, 0:1472], writes=[cfb])
        mask_f = cf[:, 0:128]; SL_f = cf[:, 128:256]; ident_f = cf[:, 256:384]; reset_f = cf[:, 384:896]
        cb16 = sb("cb16", [128, 1472], BF16); cbb = S.buf(cb16)
        S.op("vector", lambda e: e.tensor_copy(out=cb16[:, :], in_=cf[:, :]), reads=[cfb], writes=[cbb])
        mask_b = cb16[:, 0:128]; ident_b = cb16[:, 256:384]; CBd = cb16[:, 896:1408]; RT = cb16[0:64, 1408:1472]
        ones_b = sb("ones_b", [128, 128], BF16); onb = S.buf(ones_b)
        S.op("vector", lambda e: e.memset(ones_b[:, :], 1.0), writes=[onb])
        ones_f = sb("ones_f", [128, 64]); onfb = S.buf(ones_f)
        S.op("vector", lambda e: e.memset(ones_f[:, :], 1.0), writes=[onfb])
        epsc = sb("epsc", [128, 1]); epsb = S.buf(epsc)
        S.op("vector", lambda e: e.memset(epsc[:, :], EPS), writes=[epsb])
        pp_sb = sb("pp_sb", [128, 16]); ppb = S.buf(pp_sb)
        S.dma("sync", pp_sb[:, :], pp, writes=[ppb])
        bc_sb = sb("bc_sb", [128, 258]); bcb = S.buf(bc_sb)
        S.dma("sync", bc_sb[:, :], bc, writes=[bcb])
        neg_sb = sb("neg_sb", [128, 2]); negb_ = S.buf(neg_sb)
        S.op("vector", lambda e: e.tensor_scalar(out=neg_sb[:, 0:1], in0=pp_sb[:, 0:1], scalar1=-1.0, scalar2=None, op0=ALU.mult),
             reads=[ppb], pwrites=[negb_])
        S.op("vector", lambda e: e.tensor_scalar(out=neg_sb[:, 1:2], in0=bc_sb[:, 257:258], scalar1=-1.0, scalar2=None, op0=ALU.mult),
             reads=[bcb], pwrites=[negb_])
        ppc = sb("ppc", [128, 16]); ppcb = S.buf(ppc)
        S.op("vector", lambda e: e.tensor_copy(out=ppc[:, :], in_=pp_sb[:, :]), reads=[ppb], writes=[ppcb])
        S.op("vector", lambda e: e.tensor_scalar(out=ppc[:, 7:11], in0=pp_sb[:, 7:11], scalar1=0.125, scalar2=None, op0=ALU.mult), reads=[ppb], writes=[ppcb])
        up_sb = sb("up_sb", [16, 128], BF16); upb = S.buf(up_sb)
        S.dma("gpsimd", up_sb[:, :], up, writes=[upb])
        w_sb = sb("w_sb", [128, 8, NCOL], BF16); wb = S.buf(w_sb)
        for c in range(8):
            S.dma("gpsimd", w_sb[:, c, :], w[:, c, :], pwrites=[wb], sem_owner=wb)
        kaug = [sb("kaug%d" % h, [96, T], BF16) for h in range(2)]; kaugb = [S.buf(k) for k in kaug]
        for h in range(2):
            S.dma("gpsimd", kaug[h][64:96, :], onehot, pwrites=[kaugb[h]], sem_owner=kaugb[h])
        qaug = [sb("qaug%d" % h, [96, 512], BF16) for h in range(2)]; qaugb = [S.buf(q) for q in qaug]
        Vst = sb("Vst", [128, NKT, 2, 65], BF16); Vstb = S.buf(Vst)
        S.op("gpsimd", lambda e: e.memset(Vst[:, :, :, :], 1.0), writes=[Vstb])
        KmT = sb("KmT", [64, 2, 32], BF16); KmTb = S.buf(KmT)
        S.op("vector", lambda e: e.memset(KmT[:, :, :], 0.0), writes=[KmTb])
        Wst = [sb("Wst%d" % i, [128, 128], BF16) for i in range(2)]; Wstb = [S.buf(x) for x in Wst]
        S.op("vector", lambda e: e.memset(Wst[0][:, :], 0.0), writes=[Wstb[0]])
        dprev = sb("dprev", [128, 1]); dprevb = S.buf(dprev)
        S.op("vector", lambda e: e.memset(dprev[:, :], 1.0), writes=[dprevb])
        C32 = sb("C32", [64, 129]); C32b = S.buf(C32)
        Cbf = sb("Cbf", [64, 129], BF16); Cbfb = S.buf(Cbf)
        S.op("vector", lambda e: e.memset(C32[:, :], 0.0), writes=[C32b])
        S.op("vector", lambda e: e.memset(Cbf[:, :], 0.0), writes=[Cbfb])
        xq = [sb("xq%d" % i, [64, 515]) for i in range(2)]; xqb = [S.buf(x) for x in xq]
        for i in range(2):
            S.op("vector", lambda e: e.memset(xq[i][:, :], 0.0), writes=[xqb[i]])
        mb96 = sb("mb96", [128, 96], BF16); mb96b = S.buf(mb96)
        S.op("vector", lambda e: e.memset(mb96[:, :], 0.0), writes=[mb96b])
        hn_sb = [sb("hn%d" % i, [128, 8, 512], BF16) for i in range(2)]; hnb = [S.buf(x) for x in hn_sb]
        cs_sb = sb("cos_sb", [64, 2, 512]); csb = S.buf(cs_sb)
        ga_sb = sb("ga_sb", [16, 512], BF16); gab = S.buf(ga_sb)
        e1 = sb("e1", [128, 512]); e1b = S.buf(e1)
        eb = sb("eb", [128, 512]); ebb = S.buf(eb)
        enb = sb("enb", [128, 512]); enbb = S.buf(enb)
        qdec = sb("qdec", [128, 512], BF16); qdecb = S.buf(qdec)
        q2 = sb("q2", [128, 512], BF16); q2b = S.buf(q2)
        kinv = sb("kinv", [128, 512], BF16); kinvb = S.buf(kinv)
        sq = sb("sq", [64, 512], BF16); sqb = S.buf(sq)
        rs = sb("rs", [64, 512]); rsb = S.buf(rs)
        kg = sb("kg", [64, 512], BF16); kgb = S.buf(kg)
        ta = sb("ta", [64, 512]); tab = S.buf(ta)
        tb = sb("tb", [64, 512]); tbb = S.buf(tb)
        km = sb("km", [64, 2]); kmb = S.buf(km)
        gsb = sb("gsb", [128, 32]); gsbb = S.buf(gsb)
        mx8 = sb("mx8", [128, 8]); mx8b = S.buf(mx8)
        cva = sb("cva", [64, 512]); cvab = S.buf(cva)
        qc = sb("qc", [64, 512], BF16); qcb = S.buf(qc)
        kc = sb("kc", [64, 512], BF16); kcb = S.buf(kc)
        vtm = sb("vtm", [128, 4, 384], BF16); vtmb = S.buf(vtm)
        vml = sb("vml", [128, 4, 129], BF16); vmlb = S.buf(vml)
        S.op("vector", lambda e: e.memset(vml[:, :, :], 1.0), writes=[vmlb])
        ee = sb("ee", [128, 256]); eeb = S.buf(ee)
        gmo = sb("gmo", [128, 4, 128]); gmob = S.buf(gmo)
        gmg = sb("gmg", [128, 4, 128]); gmgb = S.buf(gmg)
        li = sb("li", [128, 4]); lib = S.buf(li)
        nlf = sb("nlf", [128, 4]); nlfb = S.buf(nlf)
        ytok = sb("ytok", [128, 4, 384], BF16); ytokb = S.buf(ytok)
        yT_sb = sb("yT_sb", [128, 3, 512], BF16); yTb = S.buf(yT_sb)
        Pb = [sb("P%d" % i, [128, 512], BF16) for i in range(2)]; Pbb = [S.buf(x) for x in Pb]
        kinvT = sb("kinvT", [128, 128], BF16); kinvTb = S.buf(kinvT)
        attn = sb("attn", [128, 128], BF16); attnb = S.buf(attn)
        junk = sb("junk", [128, 129], BF16); junkb = S.buf(junk)
        st1 = sb("st1", [128, 4]); st1b = S.buf(st1)
        Am = sb("Am", [128, 128]); Amb = S.buf(Am)
        DT = sb("DT", [128, 128]); DTb = S.buf(DT)
        DTm = sb("DTm", [128, 128]); DTmb = S.buf(DTm)
        sT = sb("sT", [128, 128], BF16); sTb = S.buf(sT)
        tot = sb("tot", [128, 129]); totb = S.buf(tot)
        hh = sb("hh", [128, 128]); hhb = S.buf(hh)
        kw = sb("kw", [128, 64], BF16); kwb = S.buf(kw)
        nbc = sb("nbc", [128, 64]); nbcb = S.buf(nbc)
        efl = sb("efl", [64, 1]); eflb = S.buf(efl)
        rec4 = sb("rec4", [128, 4]); rec4b = S.buf(rec4)
        accs = sb("accs", [128, 260]); accsb = S.buf(accs)
        obg = sb("obg", [128, 4, 128], BF16); obgb = S.buf(obg)
        ubg = sb("ubg", [128, 4, 128], BF16); ubgb = S.buf(ubg)
        obm = sb("obm", [128, 4, 128], BF16); obmb = S.buf(obm)
        ubm = sb("ubm", [128, 4, 128], BF16); ubmb = S.buf(ubm)
        osq = sb("osq", [128, 128], BF16); osqb = S.buf(osq)
        rsn = sb("rsn", [128, 128]); rsnb = S.buf(rsn)
        PS = [S.psum("ps%d" % i, [128, 512]) for i in range(7)]; PSb = [S.buf(p) for p in PS]
        pst = S.psum("pst", [128, 1024], BF16); pstb = S.buf(pst)
        for b_ in PSb + [pstb]:
            b_.excl = True
        rot = [0]

        def nextps():
            i = 4 + rot[0] % 3
            rot[0] += 1
            return PS[i], PSb[i]
        fmi = [0]

        def fmps():
            i = fmi[0] % 2
            fmi[0] += 1
            return PS[i], PSb[i]

        V = lambda fn, r=(), w=(), pw=(): S.op("vector", fn, reads=r, writes=w, pwrites=pw)
        A = lambda fn, r=(), w=(), pw=(): S.op("scalar", fn, reads=r, writes=w, pwrites=pw)
        G = lambda fn, r=(), w=(), pw=(): S.op("gpsimd", fn, reads=r, writes=w, pwrites=pw)
        MM = lambda fn, r=(), w=(), pw=(), inc=True: S.op("tensor", fn, reads=r, writes=w, pwrites=pw, inc=inc)

        hnTr = hnT.rearrange("(c p) t -> p c t", p=128)
        yTr = yT.rearrange("(c p) t -> p c t", p=128)
        yTdb = S.buf(yT)

        def inproj_fm(hs, hsb, col, M):
            p, pb = fmps()
            for c in range(8):
                MM(lambda e: e.matmul(p[0:M, :], lhsT=w_sb[:, c, col:col + M], rhs=hs[:, c, :], start=(c == 0), stop=(c == 7)),
                   r=[wb, hsb], w=[pb], inc=(c == 7))
            return p, pb

        def rsqrt_small(dst, dstb, src, srcb, n, scale):
            A(lambda e: e.activation(out=dst, in_=src, func=AF.Ln, scale=scale, bias=epsc[0:n, 0:1]), r=[srcb, epsb], w=[dstb])
            A(lambda e: e.activation(out=dst, in_=dst, func=AF.Exp, scale=-0.5), r=[dstb], w=[dstb])

        for t in range(NT):
            ts = t * 512
            hs, hsb = hn_sb[t % 2], hnb[t % 2]
            S.dma("sync", hs[:, :, :], hnTr[:, :, ts:ts + 512], writes=[hsb])
            S.dma("sync", cs_sb[:, 0, :], cosT[:, ts:ts + 512], pwrites=[csb], sem_owner=csb)
            S.dma("sync", cs_sb[:, 1, :], sinT[:, ts:ts + 512], pwrites=[csb], sem_owner=csb)
            if 'gd' in STAGES:
                p, pb = inproj_fm(hs, hsb, C_GA, 16)
                A(lambda e: e.copy(out=ga_sb[:, :], in_=p[0:16, :]), r=[pb], w=[gab])
                pz, pzb = nextps()
                MM(lambda e: e.matmul(pz[:, :], lhsT=up_sb[:, :], rhs=ga_sb[:, :], start=True, stop=True), r=[upb, gab], w=[pzb])
                A(lambda e: e.activation(out=e1[:, :], in_=pz[:, :], func=AF.Exp, scale=-1.0, bias=neg_sb[:, 0:1]), r=[pzb, negb_], w=[e1b])
                A(lambda e: e.activation(out=e1[:, :], in_=e1[:, :], func=AF.Ln, bias=1.0), r=[e1b], w=[e1b])
                V(lambda e: e.tensor_tensor_scan(out=e1[:, :], data0=reset_f, data1=e1[:, :], initial=0.0, op0=ALU.mult, op1=ALU.add),
                  r=[e1b, cfb], w=[e1b])
                A(lambda e: e.activation(out=eb[:, :], in_=e1[:, :], func=AF.Exp, scale=-1.0 / 16), r=[e1b], w=[ebb])
                A(lambda e: e.activation(out=enb[:, :], in_=e1[:, :], func=AF.Exp, scale=1.0 / 16), r=[e1b], w=[enbb])
                p, pb = inproj_fm(hs, hsb, C_GQ, 128)
                V(lambda e: e.scalar_tensor_tensor(out=qdec[:, :], in0=p[:, :], scalar=128 ** -0.5, in1=eb[:, :], op0=ALU.mult, op1=ALU.mult),
                  r=[pb, ebb], w=[qdecb])
                for c in range(4):
                    dcol = dprev[:, 0:1] if c == 0 else eb[:, c * 128 - 1:c * 128]
                    V(lambda e: e.tensor_scalar(out=q2[:, c * 128:(c + 1) * 128], in0=qdec[:, c * 128:(c + 1) * 128], scalar1=dcol, scalar2=None, op0=ALU.mult),
                      r=[qdecb, dprevb, ebb], pw=[q2b])
                p, pb = inproj_fm(hs, hsb, C_GK, 128)
                V(lambda e: e.tensor_tensor(out=kinv[:, :], in0=p[:, :], in1=enb[:, :], op=ALU.mult), r=[pb, enbb], w=[kinvb])
            if 'mqk' in STAGES:
                for kind in ("k", "q"):
                    for h in range(2):
                        col = (C_MKA, C_MKB)[h] if kind == "k" else (C_MQA, C_MQB)[h]
                        gcol = 2 if kind == "k" else 1
                        p, pb = inproj_fm(hs, hsb, col, 64)
                        LV = MQK_LEVEL[0]
                        if LV >= 2: A(lambda e: e.activation(out=sq[:, :], in_=p[0:64, :], func=AF.Square), r=[pb], w=[sqb])
                        pss, pssb = nextps()
                        if LV >= 3: MM(lambda e: e.matmul(pss[0:64, :], lhsT=ones_b[0:64, 0:64], rhs=sq[:, :], start=True, stop=True), r=[onb, sqb], w=[pssb])
                        if LV >= 4: A(lambda e: e.activation(out=rs[:, :], in_=pss[0:64, :], func=AF.Ln, scale=1.0 / 64, bias=epsc[0:64, 0:1]), r=[pssb, epsb], w=[rsb])
                        if LV >= 4: A(lambda e: e.activation(out=rs[:, :], in_=rs[:, :], func=AF.Exp, scale=-0.5), r=[rsb], w=[rsb])
                        if LV >= 5: A(lambda e: e.activation(out=kg[:, :], in_=p[0:64, :], func=AF.Identity, scale=pp_sb[0:64, gcol:gcol + 1]),
                          r=[pb, ppb], w=[kgb])
                        pr, prb = nextps()
                        if LV >= 6: MM(lambda e: e.matmul(pr[0:64, :], lhsT=RT, rhs=kg[:, :], start=True, stop=True), r=[cbb, kgb], w=[prb])
                        if LV >= 7: V(lambda e: e.tensor_tensor(out=ta[:, :], in0=kg[:, :], in1=cs_sb[:, 0, :], op=ALU.mult), r=[kgb, csb], w=[tab])
                        if LV >= 8: V(lambda e: e.tensor_tensor(out=tb[:, :], in0=pr[0:64, :], in1=cs_sb[:, 1, :], op=ALU.mult), r=[prb, csb], w=[tbb])
                        if LV >= 9: V(lambda e: e.tensor_tensor(out=ta[:, :], in0=ta[:, :], in1=tb[:, :], op=ALU.add), r=[tab, tbb], w=[tab])
                        if kind == "k":
                            if LV >= 10: V(lambda e: e.tensor_tensor(out=kaug[h][0:64, ts:ts + 512], in0=ta[:, :], in1=rs[:, :], op=ALU.mult),
                              r=[tab, rsb], pw=[kaugb[h]])
                            if LV >= 11: V(lambda e: e.tensor_reduce(out=km[:, :], in_=kaug[h][0:64, ts:ts + 512].rearrange("p (b k) -> p b k", k=256), axis=AX.X, op=ALU.add),
                              r=[kaugb[h]], w=[kmb])
                            if LV >= 11: V(lambda e: e.tensor_scalar(out=KmT[:, h, 2 * t:2 * t + 2], in0=km[:, :], scalar1=1.0 / 256, scalar2=None, op0=ALU.mult),
                              r=[kmb], pw=[KmTb])
                        else:
                            if LV >= 10: V(lambda e: e.scalar_tensor_tensor(out=qaug[h][0:64, :], in0=ta[:, :], scalar=0.125, in1=rs[:, :], op0=ALU.mult, op1=ALU.mult),
                              r=[tab, rsb], pw=[qaugb[h]])
                            for s in range(4 if ('gate' in STAGES and LV >= 12) else 0):
                                ob = (ts + s * 128) // 256
                                pgt, pgtb = nextps()
                                MM(lambda e: e.matmul(pgt[:, 0:32], lhsT=qaug[h][0:64, s * 128:(s + 1) * 128], rhs=KmT[:, h, :], start=True, stop=True),
                                   r=[qaugb[h], KmTb], w=[pgtb])
                                V(lambda e: e.memset(gsb[:, :], -1e30), w=[gsbb])
                                if ob > 0:
                                    V(lambda e: e.tensor_copy(out=gsb[:, 0:ob], in_=pgt[:, 0:ob]), r=[pgtb], w=[gsbb])
                                V(lambda e: e.max(out=mx8[:, :], in_=gsb[:, :]), r=[gsbb], w=[mx8b])
                                V(lambda e: e.tensor_tensor(out=gsb[:, :], in0=gsb[:, :], in1=mx8[:, 2:3].to_broadcast([128, 32]), op=ALU.is_lt),
                                  r=[gsbb, mx8b], w=[gsbb])
                                V(lambda e: e.tensor_scalar(out=mb96[:, 64:96], in0=gsb[:, :], scalar1=NEG, scalar2=None, op0=ALU.mult),
                                  r=[gsbb], w=[mb96b])
                                V(lambda e: e.memset(mb96[:, 64 + ob:65 + ob], 0.0), w=[mb96b])
                                MM(lambda e: e.transpose(out=pst[0:96, 0:128], in_=mb96[:, :], identity=ident_b), r=[mb96b, cbb], w=[pstb])
                                A(lambda e: e.copy(out=qaug[h][64:96, s * 128:(s + 1) * 128], in_=pst[64:96, 0:128]), r=[pstb], pw=[qaugb[h]])
            if 'mlc' in STAGES:
                for i, (col, dst, dstb) in enumerate(((C_LQ, qc, qcb), (C_LK, kc, kcb))):
                    p, pb = inproj_fm(hs, hsb, col, 64)
                    A(lambda e: e.copy(out=xq[i][:, 3:515], in_=p[0:64, :]), r=[pb], w=[xqb[i]])
                    c0 = 3 + 4 * i
                    A(lambda e: e.activation(out=cva[:, :], in_=xq[i][:, 0:512], func=AF.Identity, scale=ppc[0:64, c0:c0 + 1]), r=[xqb[i], ppcb], w=[cvab])
                    for wi in (1, 2, 3):
                        A(lambda e: e.activation(out=ta[:, :], in_=xq[i][:, wi:wi + 512], func=AF.Identity, scale=ppc[0:64, c0 + wi:c0 + wi + 1]), r=[xqb[i], ppcb], w=[tab])
                        if wi < 3:
                            V(lambda e: e.tensor_tensor(out=cva[:, :], in0=cva[:, :], in1=ta[:, :], op=ALU.add), r=[cvab, tab], w=[cvab])
                        else:
                            V(lambda e: e.tensor_tensor(out=dst[:, :], in0=cva[:, :], in1=ta[:, :], op=ALU.add), r=[cvab, tab], w=[dstb])
                    V(lambda e: e.tensor_copy(out=xq[i][:, 0:3], in_=xq[i][:, 512:515]), r=[xqb[i]], w=[xqb[i]])
            if 'tm' in STAGES:
                for s in range(4):
                    kt = t * 4 + s
                    for (pbank, pbk, c0, n) in ((PS[2], PSb[2], C_TM1, 384), (PS[3], PSb[3], C_TM2, 258)):
                        for c in range(8):
                            MM(lambda e: e.matmul(pbank[:, 0:n], lhsT=hs[:, c, s * 128:(s + 1) * 128], rhs=w_sb[:, c, c0:c0 + n], start=(c == 0), stop=(c == 7)),
                               r=[wb, hsb], w=[pbk], inc=(c == 7))
                    p2_, p3_ = PS[2], PS[3]
                    if TM_LEVEL[0] >= 2: A(lambda e: e.copy(out=vtm[:, s, :], in_=p2_[:, 0:384]), r=[PSb[2]], pw=[vtmb])
                    if TM_LEVEL[0] >= 3: V(lambda e: e.tensor_copy(out=Vst[:, kt, :, 0:64], in_=vtm[:, s, 128:256].rearrange("p (h d) -> p h d", d=64)), r=[vtmb], pw=[Vstb])
                    if TM_LEVEL[0] >= 4: V(lambda e: e.tensor_copy(out=vml[:, s, 0:128], in_=vtm[:, s, 256:384]), r=[vtmb], pw=[vmlb])
                    if TM_LEVEL[0] >= 5: A(lambda e: e.activation(out=ee[:, :], in_=p3_[:, 0:256], func=AF.Exp, scale=-1.0), r=[PSb[3]], w=[eeb])
                    if TM_LEVEL[0] >= 5: V(lambda e: e.tensor_scalar(out=ee[:, :], in0=ee[:, :], scalar1=1.0, scalar2=None, op0=ALU.add), r=[eeb], w=[eeb])
                    if TM_LEVEL[0] >= 5: V(lambda e: e.reciprocal(out=ee[:, :], in_=ee[:, :]), r=[eeb], w=[eeb])
                    if TM_LEVEL[0] >= 6: V(lambda e: e.tensor_tensor(out=gmo[:, s, :], in0=ee[:, 0:128], in1=bc_sb[:, 128:256], op=ALU.mult), r=[eeb, bcb], pw=[gmob])
                    if TM_LEVEL[0] >= 7: V(lambda e: e.tensor_tensor(out=gmg[:, s, :], in0=ee[:, 128:256], in1=p3_[:, 128:256], op=ALU.mult), r=[eeb, PSb[3]], pw=[gmgb])
                    if TM_LEVEL[0] >= 8: G(lambda e: e.tensor_tensor(out=gmg[:, s, :], in0=gmg[:, s, :], in1=bc_sb[:, 0:128], op=ALU.mult), r=[gmgb, bcb], pw=[gmgb])
                    if TM_LEVEL[0] >= 9: V(lambda e: e.tensor_tensor(out=li[:, s:s + 1], in0=p3_[:, 256:257], in1=bc_sb[:, 256:257], op=ALU.add), r=[PSb[3], bcb], pw=[lib])
                    if TM_LEVEL[0] >= 10: A(lambda e: e.activation(out=nlf[:, s:s + 1], in_=p3_[:, 257:258], func=AF.Exp, scale=-1.0, bias=neg_sb[:, 1:2]), r=[PSb[3], negb_], pw=[nlfb])
                    if TM_LEVEL[0] >= 10: A(lambda e: e.activation(out=nlf[:, s:s + 1], in_=nlf[:, s:s + 1], func=AF.Ln, bias=1.0), r=[nlfb], pw=[nlfb])
            def chainA():
                if 'gla' in STAGES:
                    for c in range(4):
                        cs_ = slice(c * 128, (c + 1) * 128)
                        n = t * 4 + c
                        Wold, Woldb = Wst[n % 2], Wstb[n % 2]
                        Wnew, Wnewb = Wst[(n + 1) % 2], Wstb[(n + 1) % 2]
                        dcol = dprev[:, 0:1] if c == 0 else eb[:, c * 128 - 1:c * 128]
                        if GL[0] >= 1: MM(lambda e: e.transpose(out=pst[:, 128:256], in_=kinv[:, cs_], identity=ident_b), r=[kinvb, cbb], w=[pstb])
                        yield
                        if GL[0] >= 2: A(lambda e: e.copy(out=kinvT[:, :], in_=pst[:, 128:256]), r=[pstb], w=[kinvTb])
                        yield
                        pa, pab = nextps()
                        if GL[0] >= 3: MM(lambda e: e.matmul(pa[:, 0:128], lhsT=kinv[:, cs_], rhs=qdec[:, cs_], start=True, stop=True), r=[kinvb, qdecb], w=[pab])
                        yield
                        if GL[0] >= 4: V(lambda e: e.tensor_tensor(out=attn[:, :], in0=pa[:, 0:128], in1=mask_f, op=ALU.mult), r=[pab, cfb], w=[attnb])
                        yield
                        po, pob = nextps()
                        if GL[0] >= 5: MM(lambda e: e.matmul(po[:, 0:128], lhsT=attn[:, :], rhs=vtm[:, c, 0:128], start=True, stop=False), r=[attnb, vtmb], w=[pob], inc=False)
                        yield
                        if GL[0] >= 6: MM(lambda e: e.matmul(po[:, 0:128], lhsT=q2[:, cs_], rhs=Wold[:, :], start=False, stop=True), r=[q2b, Woldb], w=[pob])
                        yield
                        if GL[0] >= 7: MM(lambda e: e.matmul(po[:, 128:256], lhsT=kinvT[:, :], rhs=vtm[:, c, 0:128], start=True, stop=True), r=[kinvTb, vtmb], w=[pob])
                        yield
                        if GL[0] >= 8: A(lambda e: e.activation(out=DTm[:, :], in_=Wold[:, :], func=AF.Identity, scale=dcol), r=[Woldb, dprevb, ebb], w=[DTmb])
                        yield
                        if GL[0] >= 9: V(lambda e: e.tensor_tensor(out=Wnew[:, :], in0=DTm[:, :], in1=po[:, 128:256], op=ALU.add), r=[DTmb, pob], w=[Wnewb])
                        yield
                        if GL[0] >= 10: A(lambda e: e.copy(out=obg[:, c, :], in_=po[:, 0:128]), r=[pob], pw=[obgb])
                        yield
                        if GL[0] >= 11: V(lambda e: e.tensor_tensor(out=ubg[:, c, :], in0=po[:, 0:128], in1=gmg[:, c, :], op=ALU.mult), r=[pob, gmgb], pw=[ubgb])
                        yield
                    if GL[0] >= 15: V(lambda e: e.tensor_copy(out=dprev[:, :], in_=eb[:, 511:512]), r=[ebb], w=[dprevb])
                    yield
                if 'ml' in STAGES:
                    for c in range(4):
                        cs_ = slice(c * 128, (c + 1) * 128)
                        A(lambda e: e.activation(out=Am[:, :], in_=SL_f, func=AF.Identity, scale=nlf[:, c:c + 1]), r=[cfb, nlfb], w=[Amb])
                        yield
                        A(lambda e: e.activation(out=nbc[:, :], in_=ones_f[:, :], func=AF.Identity, scale=nlf[:, c:c + 1]), r=[onfb, nlfb], w=[nbcb])
                        yield
                        pg, pgb = nextps()
                        MM(lambda e: e.matmul(pg[:, 0:128], lhsT=Am[:, :], rhs=mask_f, start=True, stop=True), r=[Amb, cfb], w=[pgb])
                        yield
                        MM(lambda e: e.matmul(pg[:, 128:129], lhsT=mask_f, rhs=nlf[:, c:c + 1], start=True, stop=True), r=[cfb, nlfb], w=[pgb])
                        yield
                        MM(lambda e: e.matmul(pg[0:64, 129:130], lhsT=nbc[:, :], rhs=ones_f[:, 0:1], start=True, stop=True), r=[nbcb, onfb], w=[pgb])
                        yield
                        A(lambda e: e.activation(out=DT[:, :], in_=pg[:, 0:128], func=AF.Exp, scale=-1.0, bias=li[:, c:c + 1]), r=[pgb, lib], w=[DTb])
                        yield
                        A(lambda e: e.activation(out=st1[:, 1:2], in_=pg[:, 128:129], func=AF.Exp, scale=-1.0), r=[pgb], pw=[st1b])
                        yield
                        A(lambda e: e.activation(out=efl[:, :], in_=pg[0:64, 129:130], func=AF.Exp, scale=-1.0), r=[pgb], w=[eflb])
                        yield
                        G(lambda e: e.tensor_tensor(out=DTm[:, :], in0=DT[:, :], in1=mask_f, op=ALU.mult), r=[DTb, cfb], w=[DTmb])
                        yield
                        psx, psxb = nextps()
                        MM(lambda e: e.matmul(psx[:, 0:128], lhsT=kc[:, cs_], rhs=qc[:, cs_], start=True, stop=True), r=[kcb, qcb], w=[psxb])
                        yield
                        V(lambda e: e.scalar_tensor_tensor(out=sT[:, :], in0=psx[:, 0:128], scalar=1.0, in1=DTm[:, :], op0=ALU.mult, op1=ALU.mult),
                          r=[psxb, DTmb], w=[sTb])
                        yield
                        pn, pnb = nextps()
                        MM(lambda e: e.matmul(pn[:, 0:129], lhsT=sT[:, :], rhs=vml[:, c, :], start=True, stop=True), r=[sTb, vmlb], w=[pnb])
                        yield
                        MM(lambda e: e.matmul(pn[:, 256:385], lhsT=qc[:, cs_], rhs=Cbf[:, :], start=True, stop=True), r=[qcb, Cbfb], w=[pnb])
                        yield
                        A(lambda e: e.activation(out=tot[:, :], in_=pn[:, 256:385], func=AF.Identity, scale=st1[:, 1:2]), r=[pnb, st1b], w=[totb])
                        yield
                        V(lambda e: e.tensor_tensor(out=tot[:, :], in0=tot[:, :], in1=pn[:, 0:129], op=ALU.add), r=[totb, pnb], w=[totb])
                        yield
                        A(lambda e: e.activation(out=st1[:, 2:3], in_=tot[:, 128:129], func=AF.Abs), r=[totb], pw=[st1b])
                        yield
                        V(lambda e: e.tensor_scalar(out=st1[:, 2:3], in0=st1[:, 2:3], scalar1=1.0, scalar2=None, op0=ALU.max), r=[st1b], pw=[st1b])
                        yield
                        V(lambda e: e.reciprocal(out=st1[:, 2:3], in_=st1[:, 2:3]), r=[st1b], pw=[st1b])
                        yield
                        A(lambda e: e.activation(out=hh[:, :], in_=tot[:, 0:128], func=AF.Identity, scale=st1[:, 2:3]), r=[totb, st1b], w=[hhb])
                        yield
                        A(lambda e: e.copy(out=obm[:, c, :], in_=hh[:, :]), r=[hhb], pw=[obmb])
                        yield
                        V(lambda e: e.tensor_tensor(out=ubm[:, c, :], in0=hh[:, :], in1=gmo[:, c, :], op=ALU.mult), r=[hhb, gmob], pw=[ubmb])
                        yield
                        MM(lambda e: e.transpose(out=pst[:, 256:320], in_=kc[:, cs_], identity=ident_b[0:64, 0:64]), r=[kcb, cbb], w=[pstb])
                        yield
                        A(lambda e: e.activation(out=kw[:, :], in_=pst[:, 256:320], func=AF.Identity, scale=DT[:, 127:128]), r=[pstb, DTb], w=[kwb])
                        yield
                        pk, pkb = nextps()
                        MM(lambda e: e.matmul(pk[0:64, 0:129], lhsT=kw[:, :], rhs=vml[:, c, :], start=True, stop=True), r=[kwb, vmlb], w=[pkb])
                        yield
                        A(lambda e: e.activation(out=C32[:, :], in_=C32[:, :], func=AF.Identity, scale=efl[:, 0:1]), r=[C32b, eflb], w=[C32b])
                        yield
                        V(lambda e: e.tensor_tensor(out=C32[:, :], in0=C32[:, :], in1=pk[0:64, 0:129], op=ALU.add), r=[C32b, pkb], w=[C32b])
                        yield
                        A(lambda e: e.copy(out=Cbf[:, :], in_=C32[:, :]), r=[C32b], w=[Cbfb])
                        yield

                yield
            def chainB():
                if 'moba' in STAGES:
                    for h in range(2):
                        acc, accb = PS[2 + h], PSb[2 + h]
                        nk = (t + 1) * 4
                        for kt in range(nk):
                            r_ = kt - t * 4
                            q0 = 0 if r_ < 0 else r_ * 128
                            psS, psSb = PS[kt % 2], PSb[kt % 2]
                            diag = r_ >= 0
                            MM(lambda e: e.matmul(psS[:, q0:512], lhsT=kaug[h][0:96, kt * 128:(kt + 1) * 128], rhs=qaug[h][0:96, q0:512], start=True, stop=not diag),
                               r=[kaugb[h], qaugb[h]], w=[psSb], inc=not diag)
                            if diag:
                                MM(lambda e: e.matmul(psS[:, q0:512], lhsT=ident_b, rhs=CBd[:, 0:512 - q0], start=False, stop=True), r=[cbb], w=[psSb])
                            P_, P_b = Pb[kt % 2], Pbb[kt % 2]
                            A(lambda e: e.activation(out=P_[:, q0:512], in_=psS[:, q0:512], func=AF.Exp), r=[psSb], w=[P_b])
                            for s in range(max(r_, 0), 4):
                                MM(lambda e: e.matmul(acc[:, s * 65:(s + 1) * 65], lhsT=P_[:, s * 128:(s + 1) * 128], rhs=Vst[:, kt, h, :],
                                                      start=(kt == 0 and s == 0), stop=(kt == nk - 1), skip_group_check=True),
                                   r=[P_b, Vstb], w=[accb], inc=(s == 3))
                            yield
                        A(lambda e: e.copy(out=accs[:, :], in_=acc[:, 0:260]), r=[accb], w=[accsb])
                        accv = accs[:, 0:260].rearrange("p (s d) -> p s d", d=65)
                        V(lambda e: e.reciprocal(out=rec4[:, :], in_=accv[:, :, 64]), r=[accsb], w=[rec4b])
                        for s in range(4):
                            A(lambda e: e.activation(out=ytok[:, s, 128 + h * 64:192 + h * 64], in_=accs[:, s * 65:s * 65 + 64], func=AF.Identity, scale=rec4[:, s:s + 1]),
                              r=[accsb, rec4b], pw=[ytokb])

                yield
            gens = [chainA(), chainB()]
            while gens:
                for g_ in list(gens):
                    try:
                        next(g_)
                    except StopIteration:
                        gens.remove(g_)
            if 'out' in STAGES:
                for s in range(4):
                    scol = slice(s * 128, (s + 1) * 128)
                    srcs = ((obg[:, s, :], obgb, 512), (ubg[:, s, :], ubgb, 640), (ytok[:, s, 128:256], ytokb, 768), (obm[:, s, :], obmb, 896), (ubm[:, s, :], ubmb, 384))
                    for k_, (src, srcb, off) in enumerate(srcs):
                        MM(lambda e: e.transpose(out=pst[:, off:off + 128], in_=src, identity=ident_b), r=[srcb, cbb], w=[pstb], inc=(k_ == 4))
                    for br, ooff, uoff in ((0, 512, 640), (2, 896, 384)):
                        A(lambda e: e.activation(out=osq[:, :], in_=pst[:, ooff:ooff + 128], func=AF.Square), r=[pstb], w=[osqb])
                        pss, pssb = nextps()
                        MM(lambda e: e.matmul(pss[:, 0:128], lhsT=ones_b[:, :], rhs=osq[:, :], start=True, stop=True), r=[onb, osqb], w=[pssb])
                        A(lambda e: e.activation(out=rsn[:, :], in_=pss[:, 0:128], func=AF.Ln, scale=1.0 / 128, bias=epsc[:, 0:1]), r=[pssb, epsb], w=[rsnb])
                        A(lambda e: e.activation(out=rsn[:, :], in_=rsn[:, :], func=AF.Exp, scale=-0.5), r=[rsnb], w=[rsnb])
                        V(lambda e: e.tensor_tensor(out=yT_sb[:, br, scol], in0=pst[:, uoff:uoff + 128], in1=rsn[:, :], op=ALU.mult), r=[pstb, rsnb], pw=[yTb])
                    A(lambda e: e.copy(out=yT_sb[:, 1, scol], in_=pst[:, 768:896]), r=[pstb], pw=[yTb])
                S.dma("sync", yTr[:, :, ts:ts + 512], yT_sb[:, :, :], reads=[yTb], pwrites=[yTdb], sem_owner=yTb)
        S.finish([yTdb])
    return nc


NEG = -30000.0
GLA_K = 512; MOBA_W = 512; MQK = 256; MV = 512
OFF = {}
_splits = [("g_q",512),("g_k",512),("g_v",512),("g_g",512),("g_a",16),("m_q",512),("m_k",512),("m_v",512),
           ("l_q",256),("l_k",256),("l_v",512),("l_o",512),("l_i",4),("l_f",4),("br",3072)]
_o = 0
for n, s in _splits:
    OFF[n] = _o; _o += s

def p2_cols(g):
    r = lambda name, a, b: list(range(OFF[name] + a, OFF[name] + b))
    cols = []
    cols += r("g_q", g*128, g*128+128) + r("g_k", g*128, g*128+128) + r("g_a", 0, 16)
    cols += r("m_q", g*128, g*128+64) + r("m_q", g*128+64, g*128+128) + r("m_k", g*128, g*128+64) + r("m_k", g*128+64, g*128+128)
    cols += r("l_q", g*64, g*64+64) + r("l_k", g*64, g*64+64)
    cols += r("g_v", g*128, g*128+128) + r("m_v", g*128, g*128+128) + r("l_v", g*128, g*128+128)
    cols += r("l_o", g*128, g*128+128) + r("g_g", g*128, g*128+128) + r("l_i", g, g+1) + r("l_f", g, g+1)
    assert len(cols) == 1298
    return np.array(cols)

def consts(T):
    c = np.zeros((128, 1472), np.float32)
    j = np.arange(128)[:, None]; i = np.arange(128)[None, :]
    c[:, 0:128] = (j <= i)
    c[:, 128:256] = (j > i)
    c[:, 256:384] = np.eye(128)
    rs = np.ones((128, 512), np.float32); rs[:, [0, 128, 256, 384]] = 0
    c[:, 384:896] = rs
    ii = np.arange(512)[None, :]
    c[:, 896:1408] = np.where(j <= ii, 0.0, NEG)
    RT = np.zeros((64, 64), np.float32)
    for m in range(32):
        RT[m + 32, m] = -1.0
    for m in range(32, 64):
        RT[m - 32, m] = 1.0
    c[0:64, 1408:1472] = RT
    inv = (1.0 / (np.float32(10000.0) ** (np.arange(0, 64, 2, dtype=np.float32) / np.float32(64)))).astype(np.float32)
    ang = np.arange(T, dtype=np.float32)[:, None] * inv[None, :]
    cos = np.cos(ang).astype(np.float32).T; sin = np.sin(ang).astype(np.float32).T
    cosT = np.ascontiguousarray(np.concatenate([cos, cos], 0)); sinT = np.ascontiguousarray(np.concatenate([sin, sin], 0))
    onehot = (np.arange(T)[None, :] // 256 == np.arange(32)[:, None]).astype(np.float32)
    return dict(cst=c, cosT=cosT, sinT=sinT, onehot=onehot)

def prep_p2(I, l, g):
    w_in = I["w_in"][l]
    wsel = w_in[:, p2_cols(g)]
    w = np.ascontiguousarray(wsel.reshape(8, 128, 1298).transpose(1, 0, 2))
    pp = np.zeros((128, 16), np.float32)
    pp[:, 0] = I["gla_a_b"][l][g*128:(g+1)*128]
    pp[0:64, 1] = I["moba_qn_g"][l]; pp[0:64, 2] = I["moba_kn_g"][l]
    cw = I["mlstm_conv_w"][l]
    pp[0:64, 3:7] = cw[:, g*64:(g+1)*64].T
    pp[0:64, 7:11] = cw[:, 256 + g*64:256 + (g+1)*64].T
    bc = np.zeros((128, 258), np.float32)
    bc[:, 0:128] = I["gla_norm_g"][l][None, :]; bc[:, 128:256] = I["mlstm_norm_g"][l][None, :]
    bc[:, 256] = I["mlstm_i_b"][l][g]; bc[:, 257] = I["mlstm_f_b"][l][g]
    up = np.ascontiguousarray(I["gla_a_up"][l][:, g*128:(g+1)*128])
    return dict(w=w, pp=pp, bc=bc, up=up)


from concourse.bass_utils import run_bass_kernel_spmd
import ml_dtypes

_PROG = {}


def _prog(key, fn):
    if key not in _PROG:
        _PROG[key] = fn()
    return _PROG[key]


def _tiles(w, kc):
    K, N = w.shape
    return np.ascontiguousarray(w.reshape(kc, 128, N // 128, 128).transpose(2, 1, 0, 3))


def _pp(v):
    return np.ascontiguousarray(v.reshape(-1, 128).T)


def kernel(**inputs):
    I = {k: np.asarray(v) for k, v in inputs.items()}
    x = I["x"].astype(np.float32)
    B, T, Dm = x.shape
    L = I["w_in"].shape[0]
    NTOK = T // 4
    cores = list(range(8))
    xT = [np.ascontiguousarray(x[c // 4, (c % 4) * NTOK:(c % 4 + 1) * NTOK, :].T) for c in cores]
    nc0 = _prog("p1", lambda: build_p3(NTOK, False, first_only=True))
    res = run_bass_kernel_spmd(nc0, [dict(xT=xT[c], g1n=_pp(I["norm1_g"][0])) for c in cores], core_ids=cores).results
    hn = [np.asarray(res[c]["hno"]) for c in cores]
    cst = consts(T)
    nc2 = _prog("p2", lambda: build_p2(T))
    for l in range(L):
        last = (l == L - 1)
        in2 = []
        for c in cores:
            b, g = c // 4, c % 4
            d = prep_p2(I, l, g)
            d.update(cst)
            d["hnT"] = np.ascontiguousarray(np.concatenate([hn[b * 4 + j] for j in range(4)], axis=1))
            in2.append(d)
        res2 = run_bass_kernel_spmd(nc2, in2, core_ids=cores).results
        yT = [np.asarray(res2[c]["yT"]) for c in cores]
        wl = dict(wg=_tiles(np.ascontiguousarray(I["w_in"][l][:, OFF["br"]:]), 8), gbias=_pp(I["gate_b"][l]),
                  wbr=np.concatenate([_tiles(I["w_br_gla"][l], 4), _tiles(I["w_br_moba"][l], 4), _tiles(I["w_br_mlstm"][l], 4)], 0),
                  wo=_tiles(I["w_out"][l], 8), g2=_pp(I["norm2_g"][l]), w1=_tiles(I["w_ff1"][l], 8), w2=_tiles(I["w_ff2"][l], 32),
                  g1n=_pp(I["norm1_g"][min(l + 1, L - 1)]))
        nc3 = _prog("p3_%d" % last, lambda: build_p3(NTOK, last))
        in3 = []
        for c in cores:
            b, j = c // 4, c % 4
            d = dict(wl)
            d["xT"] = xT[c]
            d["hnT"] = hn[c]
            d["yT"] = np.ascontiguousarray(np.concatenate([yT[b * 4 + g][:, j * NTOK:(j + 1) * NTOK] for g in range(4)], axis=0))
            in3.append(d)
        res3 = run_bass_kernel_spmd(nc3, in3, core_ids=cores).results
        xT = [np.asarray(res3[c]["xo"]) for c in cores]
        if not last:
            hn = [np.asarray(res3[c]["hno"]) for c in cores]
    out = np.empty((B, T, Dm), np.float32)
    for c in cores:
        out[c // 4, (c % 4) * NTOK:(c % 4 + 1) * NTOK, :] = xT[c].T
    return out
```

```python
import numpy as np
import concourse.bass as bass
import concourse.mybir as mybir
from contextlib import ExitStack

F32 = mybir.dt.float32
BF16 = mybir.dt.bfloat16
AF = mybir.ActivationFunctionType
ALU = mybir.AluOpType
AX = mybir.AxisListType


class Buf:
    __slots__ = ("ap", "w", "r", "dsem", "dcnt", "name", "excl")

    def __init__(self, ap, name=""):
        self.ap = ap
        self.w = {}
        self.r = {}
        self.dsem = None
        self.dcnt = 0
        self.name = name
        self.excl = False

    def __getitem__(self, idx):
        return self.ap[idx]


class EngW:
    def __init__(self, S, name):
        self.S = S
        self.name = name
        self.e = getattr(S.nc, name)
        self.sem = S.stack.enter_context(S.nc.semaphore("es_" + name))
        self.cnt = 0
        self.waited = {}

    def wait(self, ev):
        if ev is None:
            return
        sem, val = ev
        k = id(sem)
        if self.waited.get(k, 0) >= val:
            return
        self.e.wait_ge(sem, val)
        self.waited[k] = val


class Sched:
    def __init__(self, nc, stack):
        self.nc = nc
        self.stack = stack
        self.E = {n: EngW(self, n) for n in ("tensor", "vector", "scalar", "gpsimd", "sync")}
        self.nsem = 5
        self.pending = {}

    def sbuf(self, name, shape, dt):
        t = self.stack.enter_context(self.nc.sbuf_tensor(name, list(shape), dt))
        return t

    def psum(self, name, shape, dt=F32):
        t = self.stack.enter_context(self.nc.psum_tensor(name, list(shape), dt))
        return t

    def buf(self, ap, name=""):
        return Buf(ap, name)

    def newsem(self, name):
        self.nsem += 1
        return self.stack.enter_context(self.nc.semaphore(name))

    def _deps(self, E, reads, writes, pwrites, same_ok, skip_sem=None):
        for b in reads:
            for ev in b.w.values():
                E.wait(ev)
            if b.excl:
                for ev in b.r.values():
                    if ev[0] is not E.sem:
                        E.wait(ev)
        for b in writes:
            for ev in b.w.values():
                if not (same_ok and ev[0] is E.sem):
                    E.wait(ev)
            for ev in b.r.values():
                if not (same_ok and ev[0] is E.sem):
                    E.wait(ev)
        for b in pwrites:
            for ev in b.w.values():
                if ev[0] is not E.sem and ev[0] is not skip_sem:
                    E.wait(ev)
            for ev in b.r.values():
                if not (same_ok and ev[0] is E.sem):
                    E.wait(ev)

    def _record(self, ev, reads, writes, pwrites):
        k = id(ev[0])
        for b in reads:
            b.r[k] = ev
        for b in writes:
            b.w = {k: ev}
            b.r = {}
        for b in pwrites:
            b.w[k] = ev

    def op(self, eng, fn, reads=(), writes=(), pwrites=(), inc=True):
        E = self.E[eng]
        self._deps(E, reads, writes, pwrites, eng == "tensor")
        ins = fn(E.e)
        if inc:
            E.cnt += 1
            ins.then_inc(E.sem, 1)
            ev = (E.sem, E.cnt)
        else:
            ev = (E.sem, E.cnt + 1)
        self._record(ev, reads, writes, pwrites)
        return ev

    def dma(self, q, out_ap, in_ap, reads=(), writes=(), pwrites=(), sem_owner=None, **kw):
        E = self.E[q]
        owner = sem_owner or (writes[0] if writes else (pwrites[0] if pwrites else reads[0]))
        if owner.dsem is None:
            owner.dsem = self.newsem("ds_%d" % self.nsem)
        self._deps(E, reads, writes, pwrites, False, skip_sem=owner.dsem)
        ins = E.e.dma_start(out=out_ap, in_=in_ap, **kw)
        owner.dcnt += 16
        ins.then_inc(owner.dsem, 16)
        ev = (owner.dsem, owner.dcnt)
        self._record(ev, reads, writes, pwrites)
        return ev

    def finish(self, bufs, eng="sync"):
        E = self.E[eng]
        for b in bufs:
            for ev in b.w.values():
                E.wait(ev)
            for ev in b.r.values():
                E.wait(ev)


EPS = 1e-6


def emit_norm(S, K, x_sb, xb, g_sb, gb, out_sb, outb, ntok, ps_ss, ps_ssb, sq_sb, sqb, tmp_sb, tmpb):
    nc = S.nc
    for h in range(ntok // 512):
        hs = slice(h * 512, (h + 1) * 512)
        S.op("scalar", lambda e: e.activation(out=sq_sb[:, :, :], in_=x_sb[:, :, hs], func=AF.Square),
             reads=[xb], writes=[sqb])
        for c in range(8):
            S.op("tensor", lambda e: e.matmul(ps_ss[:, :], lhsT=K["ones"][:, :], rhs=sq_sb[:, c, :],
                                              start=(c == 0), stop=(c == 7)),
                 reads=[K["onesb"], sqb], writes=[ps_ssb], inc=(c == 7))
        S.op("scalar", lambda e: e.activation(out=tmp_sb[:, :], in_=ps_ss[:, :], func=AF.Ln, scale=1.0 / 1024, bias=K["eps"][:, 0:1]),
             reads=[ps_ssb, K["epsb"]], writes=[tmpb])
        S.op("scalar", lambda e: e.activation(out=tmp_sb[:, :], in_=tmp_sb[:, :], func=AF.Exp, scale=-0.5),
             reads=[tmpb], writes=[tmpb])
        for c in range(8):
            S.op("vector", lambda e: e.scalar_tensor_tensor(out=out_sb[:, c, hs], in0=x_sb[:, c, hs], scalar=g_sb[:, c:c + 1],
                                                            in1=tmp_sb[:, :], op0=ALU.mult, op1=ALU.mult),
                 reads=[xb, gb, tmpb], pwrites=[outb])


def build_p3(ntok, last, first_only=False, stop_after='Z'):
    nc = bass.Bass("TRN2", target_bir_lowering=False)
    D = lambda name, shape, dt=F32, kind="ExternalInput": nc.dram_tensor(name, list(shape), dt, kind=kind).ap()
    xT = D("xT", [1024, ntok])
    g1n = D("g1n", [128, 8])
    if not first_only:
        hnT = D("hnT", [1024, ntok], BF16)
        yT = D("yT", [1536, ntok], BF16)
        wg = D("wg", [24, 128, 8, 128]); gbias = D("gbias", [128, 24])
        wbr = D("wbr", [24, 128, 4, 128])
        wo = D("wo", [8, 128, 8, 128])
        g2 = D("g2", [128, 8])
        w1 = D("w1", [32, 128, 8, 128]); w2 = D("w2", [8, 128, 32, 128])
        xo = D("xo", [1024, ntok], F32, "ExternalOutput")
    if not last:
        hno = D("hno", [1024, ntok], BF16, "ExternalOutput")
    G = 1024
    with ExitStack() as st:
        S = Sched(nc, st)
        K = {}
        K["ones"] = S.sbuf("ones", [128, 128], BF16); K["onesb"] = S.buf(K["ones"])
        K["eps"] = S.sbuf("epsc", [128, 1], F32); K["epsb"] = S.buf(K["eps"])
        S.op("vector", lambda e: e.memset(K["ones"][:, :], 1.0), writes=[K["onesb"]])
        S.op("vector", lambda e: e.memset(K["eps"][:, :], EPS), writes=[K["epsb"]])
        x_sb = S.sbuf("x_sb", [128, 8, G], F32); xb = S.buf(x_sb)
        hn_sb = S.sbuf("hn_sb", [128, 8, G], BF16); hnb = S.buf(hn_sb)
        g1_sb = S.sbuf("g1_sb", [128, 8], F32); g1b = S.buf(g1_sb)
        sq_sb = S.sbuf("sq_sb", [128, 8, 512], BF16); sqb = S.buf(sq_sb)
        tmp_sb = S.sbuf("tmp_sb", [128, 512], F32); tmpb = S.buf(tmp_sb)
        PS = [S.psum("ps%d" % i, [128, 512]) for i in range(8)]
        PSb = [S.buf(p) for p in PS]
        S.dma("sync", g1_sb[:, :], g1n, writes=[g1b])
        xTr = xT.rearrange("(c p) t -> p c t", p=128)
        if not last:
            hnor = hno.rearrange("(c p) t -> p c t", p=128)
            hnob = S.buf(hno)
        if first_only:
            for gi in range(ntok // G):
                gs = slice(gi * G, (gi + 1) * G)
                S.dma("sync", x_sb[:, :, :], xTr[:, :, gs], writes=[xb])
                emit_norm(S, K, x_sb, xb, g1_sb, g1b, hn_sb, hnb, G, PS[0], PSb[0], sq_sb, sqb, tmp_sb, tmpb)
                S.dma("sync", hnor[:, :, gs], hn_sb[:, :, :], reads=[hnb], pwrites=[hnob], sem_owner=hnb)
            S.finish([hnob])
            return nc
        mixed = S.sbuf("mixed", [128, 8, G], BF16); mixb = S.buf(mixed)
        big = S.sbuf("big", [128, 32, G], BF16); bigb = S.buf(big)
        NW = 3
        wt = [S.sbuf("wt%d" % i, [128, 32 * 128], BF16) for i in range(NW)]
        wtb = [S.buf(w) for w in wt]
        wctr = [0]
        gb_sb = S.sbuf("gb_sb", [128, 24], F32); gbb = S.buf(gb_sb)
        g2_sb = S.sbuf("g2_sb", [128, 8], F32); g2b = S.buf(g2_sb)
        acc = S.sbuf("acc", [128, G], F32); accb = S.buf(acc)
        t1 = [S.sbuf("t1_%d" % i, [128, 512], F32) for i in range(2)]; t1b = [S.buf(t) for t in t1]
        S.dma("sync", gb_sb[:, :], gbias, writes=[gbb])
        S.dma("sync", g2_sb[:, :], g2, writes=[g2b])
        S.op("vector", lambda e: e.tensor_scalar(out=gb_sb[:, :], in0=gb_sb[:, :], scalar1=-1.0, scalar2=None, op0=ALU.mult),
             reads=[gbb], writes=[gbb])
        xob = S.buf(xo)
        xor_ = xo.rearrange("(c p) t -> p c t", p=128)
        hnTr = hnT.rearrange("(c p) t -> p c t", p=128)
        yTr = yT.rearrange("(c p) t -> p c t", p=128)

        def wtile(src, kc):
            i = wctr[0] % NW
            wctr[0] += 1
            view = wt[i][:, 0:kc * 128].rearrange("p (k n) -> p k n", n=128)
            S.dma("gpsimd", view, src, writes=[wtb[i]])
            return view, wtb[i]

        psi = [0]

        def nextps():
            i = psi[0] % 6
            psi[0] += 1
            return PS[i], PSb[i]

        tci = [0]
        for gi in range(ntok // G):
            gs = slice(gi * G, (gi + 1) * G)
            S.dma("sync", x_sb[:, :, :], xTr[:, :, gs], writes=[xb])
            S.dma("sync", hn_sb[:, :, :], hnTr[:, :, gs], writes=[hnb])
            S.dma("sync", big[:, 0:12, :], yTr[:, :, gs], writes=[bigb])
            for m in range(8 if stop_after >= 'B' else 0):
                for br in range(3):
                    j = br * 8 + m
                    wgt, wgtb = wtile(wg[j], 8)
                    wbt, wbtb = wtile(wbr[j], 4)
                    for h in range(2):
                        hs = slice(h * 512, (h + 1) * 512)
                        pg, pgb = nextps()
                        for c in range(8):
                            S.op("tensor", lambda e: e.matmul(pg[:, :], lhsT=wgt[:, c, :], rhs=hn_sb[:, c, hs], start=(c == 0), stop=(c == 7)),
                                 reads=[wgtb, hnb], writes=[pgb], inc=(c == 7))
                        pb, pbb = nextps()
                        for g in range(4):
                            S.op("tensor", lambda e: e.matmul(pb[:, :], lhsT=wbt[:, g, :], rhs=big[:, g * 3 + br, hs], start=(g == 0), stop=(g == 3)),
                                 reads=[wbtb, bigb], writes=[pbb], inc=(g == 3))
                        ti = tci[0] % 2; tci[0] += 1
                        tt, ttb = t1[ti], t1b[ti]
                        S.op("scalar", lambda e: e.activation(out=tt[:, :], in_=pg[:, :], func=AF.Exp, scale=-1.0, bias=gb_sb[:, j:j + 1]),
                             reads=[pgb, gbb], writes=[ttb])
                        S.op("scalar", lambda e: e.activation(out=tt[:, :], in_=tt[:, :], func=AF.Ln, bias=1.0),
                             reads=[ttb], writes=[ttb])
                        S.op("scalar", lambda e: e.activation(out=tt[:, :], in_=tt[:, :], func=AF.Exp, scale=-1.0),
                             reads=[ttb], writes=[ttb])
                        if br == 0:
                            S.op("vector", lambda e: e.tensor_tensor(out=acc[:, hs], in0=tt[:, :], in1=pb[:, :], op=ALU.mult),
                                 reads=[ttb, pbb], pwrites=[accb])
                        else:
                            S.op("vector", lambda e: e.tensor_tensor(out=tt[:, :], in0=tt[:, :], in1=pb[:, :], op=ALU.mult),
                                 reads=[ttb, pbb], writes=[ttb])
                            if br == 1:
                                S.op("vector", lambda e: e.tensor_tensor(out=acc[:, hs], in0=acc[:, hs], in1=tt[:, :], op=ALU.add),
                                     reads=[ttb, accb], pwrites=[accb])
                            else:
                                S.op("vector", lambda e: e.tensor_tensor(out=mixed[:, m, hs], in0=acc[:, hs], in1=tt[:, :], op=ALU.add),
                                     reads=[ttb, accb], pwrites=[mixb])
            for m in range(8 if stop_after >= 'C' else 0):
                wot, wotb = wtile(wo[m], 8)
                for h in range(2):
                    hs = slice(h * 512, (h + 1) * 512)
                    p, pb_ = nextps()
                    for c in range(8):
                        S.op("tensor", lambda e: e.matmul(p[:, :], lhsT=wot[:, c, :], rhs=mixed[:, c, hs], start=(c == 0), stop=(c == 7)),
                             reads=[wotb, mixb], writes=[pb_], inc=(c == 7))
                    S.op("vector", lambda e: e.tensor_tensor(out=x_sb[:, m, hs], in0=x_sb[:, m, hs], in1=p[:, :], op=ALU.add),
                         reads=[pb_, xb], pwrites=[xb])
            emit_norm(S, K, x_sb, xb, g2_sb, g2b, hn_sb, hnb, G, PS[6], PSb[6], sq_sb, sqb, tmp_sb, tmpb)
            for f in range(32 if stop_after >= 'E' else 0):
                w1t, w1tb = wtile(w1[f], 8)
                for h in range(2):
                    hs = slice(h * 512, (h + 1) * 512)
                    p, pb_ = nextps()
                    for c in range(8):
                        S.op("tensor", lambda e: e.matmul(p[:, :], lhsT=w1t[:, c, :], rhs=hn_sb[:, c, hs], start=(c == 0), stop=(c == 7)),
                             reads=[w1tb, hnb], writes=[pb_], inc=(c == 7))
                    ti = tci[0] % 2; tci[0] += 1
                    tt, ttb = t1[ti], t1b[ti]
                    S.op("scalar", lambda e: e.activation(out=tt[:, :], in_=p[:, :], func=AF.Relu), reads=[pb_], writes=[ttb])
                    S.op("vector", lambda e: e.tensor_tensor(out=big[:, f, hs], in0=tt[:, :], in1=tt[:, :], op=ALU.mult),
                         reads=[ttb], pwrites=[bigb])
            for m in range(8 if stop_after >= 'F' else 0):
                w2t, w2tb = wtile(w2[m], 32)
                for h in range(2):
                    hs = slice(h * 512, (h + 1) * 512)
                    p, pb_ = nextps()
                    for f in range(32):
                        S.op("tensor", lambda e: e.matmul(p[:, :], lhsT=w2t[:, f, :], rhs=big[:, f, hs], start=(f == 0), stop=(f == 31)),
                             reads=[w2tb, bigb], writes=[pb_], inc=(f == 31))
                    S.op("vector", lambda e: e.tensor_tensor(out=x_sb[:, m, hs], in0=x_sb[:, m, hs], in1=p[:, :], op=ALU.add),
                         reads=[pb_, xb], pwrites=[xb])
            S.dma("sync", xor_[:, :, gs], x_sb[:, :, :], reads=[xb], pwrites=[xob], sem_owner=xb)
            if not last:
                emit_norm(S, K, x_sb, xb, g1_sb, g1b, hn_sb, hnb, G, PS[6], PSb[6], sq_sb, sqb, tmp_sb, tmpb)
                S.dma("sync", hnor[:, :, gs], hn_sb[:, :, :], reads=[hnb], pwrites=[hnob], sem_owner=hnb)
        S.finish([xob] + ([hnob] if not last else []))
    return nc


EPS = 1e-6
NEG = -30000.0
NCOL = 1298
MQK_LEVEL = [99]
GL = [99]
TM_LEVEL = [99]
STAGES = ['gd', 'mqk', 'gate', 'mlc', 'tm', 'gla', 'ml', 'moba', 'out']
C_GQ, C_GK, C_GA, C_MQA, C_MQB, C_MKA, C_MKB, C_LQ, C_LK = 0, 128, 256, 272, 336, 400, 464, 528, 592
C_TM1, C_TM2 = 656, 1040


def build_p2(T):
    nc = bass.Bass("TRN2", target_bir_lowering=False)
    D = lambda name, shape, dt=F32, kind="ExternalInput": nc.dram_tensor(name, list(shape), dt, kind=kind).ap()
    NT = T // 512
    NKT = T // 128
    NB = T // 256
    hnT = D("hnT", [1024, T], BF16)
    w = D("w", [128, 8, NCOL])
    pp = D("pp", [128, 16])
    bc = D("bc", [128, 258])
    up = D("up", [16, 128])
    cosT = D("cosT", [64, T]); sinT = D("sinT", [64, T])
    cst = D("cst", [128, 1472])
    onehot = D("onehot", [32, T])
    yT = D("yT", [384, T], BF16, "ExternalOutput")
    with ExitStack() as st:
        S = Sched(nc, st)
        sb = lambda name, shape, dt=F32: S.sbuf(name, shape, dt)
        cf = sb("cf", [128, 1472]); cfb = S.buf(cf)
        S.dma("sync", cf[:, :], cst[:, 0:1472], writes=[cfb])
        mask_f = cf[:, 0:128]; SL_f = cf[:, 128:256]; ident_f = cf[:, 256:384]; reset_f = cf[:, 384:896]
        cb16 = sb("cb16", [128, 1472], BF16); cbb = S.buf(cb16)
        S.op("vector", lambda e: e.tensor_copy(out=cb16[:, :], in_=cf[:, :]), reads=[cfb], writes=[cbb])
        mask_b = cb16[:, 0:128]; ident_b = cb16[:, 256:384]; CBd = cb16[:, 896:1408]; RT = cb16[0:64, 1408:1472]
        ones_b = sb("ones_b", [128, 128], BF16); onb = S.buf(ones_b)
        S.op("vector", lambda e: e.memset(ones_b[:, :], 1.0), writes=[onb])
        ones_f = sb("ones_f", [128, 64]); onfb = S.buf(ones_f)
        S.op("vector", lambda e: e.memset(ones_f[:, :], 1.0), writes=[onfb])
        epsc = sb("epsc", [128, 1]); epsb = S.buf(epsc)
        S.op("vector", lambda e: e.memset(epsc[:, :], EPS), writes=[epsb])
        pp_sb = sb("pp_sb", [128, 16]); ppb = S.buf(pp_sb)
        S.dma("sync", pp_sb[:, :], pp, writes=[ppb])
        bc_sb = sb("bc_sb", [128, 258]); bcb = S.buf(bc_sb)
        S.dma("sync", bc_sb[:, :], bc, writes=[bcb])
        neg_sb = sb("neg_sb", [128, 2]); negb_ = S.buf(neg_sb)
        S.op("vector", lambda e: e.tensor_scalar(out=neg_sb[:, 0:1], in0=pp_sb[:, 0:1], scalar1=-1.0, scalar2=None, op0=ALU.mult),
             reads=[ppb], pwrites=[negb_])
        S.op("vector", lambda e: e.tensor_scalar(out=neg_sb[:, 1:2], in0=bc_sb[:, 257:258], scalar1=-1.0, scalar2=None, op0=ALU.mult),
             reads=[bcb], pwrites=[negb_])
        ppc = sb("ppc", [128, 16]); ppcb = S.buf(ppc)
        S.op("vector", lambda e: e.tensor_copy(out=ppc[:, :], in_=pp_sb[:, :]), reads=[ppb], writes=[ppcb])
        S.op("vector", lambda e: e.tensor_scalar(out=ppc[:, 7:11], in0=pp_sb[:, 7:11], scalar1=0.125, scalar2=None, op0=ALU.mult), reads=[ppb], writes=[ppcb])
        up_sb = sb("up_sb", [16, 128], BF16); upb = S.buf(up_sb)
        S.dma("gpsimd", up_sb[:, :], up, writes=[upb])
        w_sb = sb("w_sb", [128, 8, NCOL], BF16); wb = S.buf(w_sb)
        for c in range(8):
            S.dma("gpsimd", w_sb[:, c, :], w[:, c, :], pwrites=[wb], sem_owner=wb)
        kaug = [sb("kaug%d" % h, [96, T], BF16) for h in range(2)]; kaugb = [S.buf(k) for k in kaug]
        for h in range(2):
            S.dma("gpsimd", kaug[h][64:96, :], onehot, pwrites=[kaugb[h]], sem_owner=kaugb[h])
        qaug = [sb("qaug%d" % h, [96, 512], BF16) for h in range(2)]; qaugb = [S.buf(q) for q in qaug]
        Vst = sb("Vst", [128, NKT, 2, 65], BF16); Vstb = S.buf(Vst)
        S.op("gpsimd", lambda e: e.memset(Vst[:, :, :, :], 1.0), writes=[Vstb])
        KmT = sb("KmT", [64, 2, 32], BF16); KmTb = S.buf(KmT)
        S.op("vector", lambda e: e.memset(KmT[:, :, :], 0.0), writes=[KmTb])
        Wst = [sb("Wst%d" % i, [128, 128], BF16) for i in range(2)]; Wstb = [S.buf(x) for x in Wst]
        S.op("vector", lambda e: e.memset(Wst[0][:, :], 0.0), writes=[Wstb[0]])
        dprev = sb("dprev", [128, 1]); dprevb = S.buf(dprev)
        S.op("vector", lambda e: e.memset(dprev[:, :], 1.0), writes=[dprevb])
        C32 = sb("C32", [64, 129]); C32b = S.buf(C32)
        Cbf = sb("Cbf", [64, 129], BF16); Cbfb = S.buf(Cbf)
        S.op("vector", lambda e: e.memset(C32[:, :], 0.0), writes=[C32b])
        S.op("vector", lambda e: e.memset(Cbf[:, :], 0.0), writes=[Cbfb])
        xq = [sb("xq%d" % i, [64, 515]) for i in range(2)]; xqb = [S.buf(x) for x in xq]
        for i in range(2):
            S.op("vector", lambda e: e.memset(xq[i][:, :], 0.0), writes=[xqb[i]])
        mb96 = sb("mb96", [128, 96], BF16); mb96b = S.buf(mb96)
        S.op("vector", lambda e: e.memset(mb96[:, :], 0.0), writes=[mb96b])
        hn_sb = [sb("hn%d" % i, [128, 8, 512], BF16) for i in range(2)]; hnb = [S.buf(x) for x in hn_sb]
        cs_sb = sb("cos_sb", [64, 2, 512]); csb = S.buf(cs_sb)
        ga_sb = sb("ga_sb", [16, 512], BF16); gab = S.buf(ga_sb)
        e1 = sb("e1", [128, 512]); e1b = S.buf(e1)
        eb = sb("eb", [128, 512]); ebb = S.buf(eb)
        enb = sb("enb", [128, 512]); enbb = S.buf(enb)
        qdec = sb("qdec", [128, 512], BF16); qdecb = S.buf(qdec)
        q2 = sb("q2", [128, 512], BF16); q2b = S.buf(q2)
        kinv = sb("kinv", [128, 512], BF16); kinvb = S.buf(kinv)
        sq = sb("sq", [64, 512], BF16); sqb = S.buf(sq)
        rs = sb("rs", [64, 512]); rsb = S.buf(rs)
        kg = sb("kg", [64, 512], BF16); kgb = S.buf(kg)
        ta = sb("ta", [64, 512]); tab = S.buf(ta)
        tb = sb("tb", [64, 512]); tbb = S.buf(tb)
        km = sb("km", [64, 2]); kmb = S.buf(km)
        gsb = sb("gsb", [128, 32]); gsbb = S.buf(gsb)
        mx8 = sb("mx8", [128, 8]); mx8b = S.buf(mx8)
        cva = sb("cva", [64, 512]); cvab = S.buf(cva)
        qc = sb("qc", [64, 512], BF16); qcb = S.buf(qc)
        kc = sb("kc", [64, 512], BF16); kcb = S.buf(kc)
        vtm = sb("vtm", [128, 4, 384], BF16); vtmb = S.buf(vtm)
        vml = sb("vml", [128, 4, 129], BF16); vmlb = S.buf(vml)
        S.op("vector", lambda e: e.memset(vml[:, :, :], 1.0), writes=[vmlb])
        ee = sb("ee", [128, 256]); eeb = S.buf(ee)
        gmo = sb("gmo", [128, 4, 128]); gmob = S.buf(gmo)
        gmg = sb("gmg", [128, 4, 128]); gmgb = S.buf(gmg)
        li = sb("li", [128, 4]); lib = S.buf(li)
        nlf = sb("nlf", [128, 4]); nlfb = S.buf(nlf)
        ytok = sb("ytok", [128, 4, 384], BF16); ytokb = S.buf(ytok)
        yT_sb = sb("yT_sb", [128, 3, 512], BF16); yTb = S.buf(yT_sb)
        Pb = [sb("P%d" % i, [128, 512], BF16) for i in range(2)]; Pbb = [S.buf(x) for x in Pb]
        kinvT = sb("kinvT", [128, 128], BF16); kinvTb = S.buf(kinvT)
        attn = sb("attn", [128, 128], BF16); attnb = S.buf(attn)
        junk = sb("junk", [128, 129], BF16); junkb = S.buf(junk)
        st1 = sb("st1", [128, 4]); st1b = S.buf(st1)
        Am = sb("Am", [128, 128]); Amb = S.buf(Am)
        DT = sb("DT", [128, 128]); DTb = S.buf(DT)
        DTm = sb("DTm", [128, 128]); DTmb = S.buf(DTm)
        Wtmp = sb("Wtmp", [128, 128]); Wtmpb = S.buf(Wtmp)
        sT = sb("sT", [128, 128], BF16); sTb = S.buf(sT)
        tot = sb("tot", [128, 129]); totb = S.buf(tot)
        hh = sb("hh", [128, 128]); hhb = S.buf(hh)
        kw = sb("kw", [128, 64], BF16); kwb = S.buf(kw)
        nbc = sb("nbc", [128, 64]); nbcb = S.buf(nbc)
        efl = sb("efl", [64, 1]); eflb = S.buf(efl)
        rec4 = sb("rec4", [128, 4]); rec4b = S.buf(rec4)
        accs = sb("accs", [128, 260]); accsb = S.buf(accs)
        obg = sb("obg", [128, 4, 128], BF16); obgb = S.buf(obg)
        ubg = sb("ubg", [128, 4, 128], BF16); ubgb = S.buf(ubg)
        obm = sb("obm", [128, 4, 128], BF16); obmb = S.buf(obm)
        ubm = sb("ubm", [128, 4, 128], BF16); ubmb = S.buf(ubm)
        osq = sb("osq", [128, 128], BF16); osqb = S.buf(osq)
        rsn = sb("rsn", [128, 128]); rsnb = S.buf(rsn)
        PS = [S.psum("ps%d" % i, [128, 512]) for i in range(7)]; PSb = [S.buf(p) for p in PS]
        pst = S.psum("pst", [128, 1024], BF16); pstb = S.buf(pst)
        for b_ in PSb + [pstb]:
            b_.excl = True
        rot = [0]

        def nextps():
            i = 4 + rot[0] % 3
            rot[0] += 1
            return PS[i], PSb[i]
        fmi = [0]

        def fmps():
            i = fmi[0] % 2
            fmi[0] += 1
            return PS[i], PSb[i]

        V = lambda fn, r=(), w=(), pw=(): S.op("vector", fn, reads=r, writes=w, pwrites=pw)
        A = lambda fn, r=(), w=(), pw=(): S.op("scalar", fn, reads=r, writes=w, pwrites=pw)
        G = lambda fn, r=(), w=(), pw=(): S.op("gpsimd", fn, reads=r, writes=w, pwrites=pw)
        MM = lambda fn, r=(), w=(), pw=(), inc=True: S.op("tensor", fn, reads=r, writes=w, pwrites=pw, inc=inc)

        hnTr = hnT.rearrange("(c p) t -> p c t", p=128)
        yTr = yT.rearrange("(c p) t -> p c t", p=128)
        yTdb = S.buf(yT)

        def inproj_fm(hs, hsb, col, M):
            p, pb = fmps()
            for c in range(8):
                MM(lambda e: e.matmul(p[0:M, :], lhsT=w_sb[:, c, col:col + M], rhs=hs[:, c, :], start=(c == 0), stop=(c == 7)),
                   r=[wb, hsb], w=[pb], inc=(c == 7))
            return p, pb

        def rsqrt_small(dst, dstb, src, srcb, n, scale):
            A(lambda e: e.activation(out=dst, in_=src, func=AF.Ln, scale=scale, bias=epsc[0:n, 0:1]), r=[srcb, epsb], w=[dstb])
            A(lambda e: e.activation(out=dst, in_=dst, func=AF.Exp, scale=-0.5), r=[dstb], w=[dstb])

        for t in range(NT):
            ts = t * 512
            hs, hsb = hn_sb[t % 2], hnb[t % 2]
            S.dma("sync", hs[:, :, :], hnTr[:, :, ts:ts + 512], writes=[hsb])
            S.dma("sync", cs_sb[:, 0, :], cosT[:, ts:ts + 512], pwrites=[csb], sem_owner=csb)
            S.dma("sync", cs_sb[:, 1, :], sinT[:, ts:ts + 512], pwrites=[csb], sem_owner=csb)
            if 'gd' in STAGES:
                p, pb = inproj_fm(hs, hsb, C_GA, 16)
                A(lambda e: e.copy(out=ga_sb[:, :], in_=p[0:16, :]), r=[pb], w=[gab])
                pz, pzb = nextps()
                MM(lambda e: e.matmul(pz[:, :], lhsT=up_sb[:, :], rhs=ga_sb[:, :], start=True, stop=True), r=[upb, gab], w=[pzb])
                A(lambda e: e.activation(out=e1[:, :], in_=pz[:, :], func=AF.Exp, scale=-1.0, bias=neg_sb[:, 0:1]), r=[pzb, negb_], w=[e1b])
                A(lambda e: e.activation(out=e1[:, :], in_=e1[:, :], func=AF.Ln, bias=1.0), r=[e1b], w=[e1b])
                V(lambda e: e.tensor_tensor_scan(out=e1[:, :], data0=reset_f, data1=e1[:, :], initial=0.0, op0=ALU.mult, op1=ALU.add),
                  r=[e1b, cfb], w=[e1b])
                A(lambda e: e.activation(out=eb[:, :], in_=e1[:, :], func=AF.Exp, scale=-1.0 / 16), r=[e1b], w=[ebb])
                A(lambda e: e.activation(out=enb[:, :], in_=e1[:, :], func=AF.Exp, scale=1.0 / 16), r=[e1b], w=[enbb])
                p, pb = inproj_fm(hs, hsb, C_GQ, 128)
                V(lambda e: e.scalar_tensor_tensor(out=qdec[:, :], in0=p[:, :], scalar=128 ** -0.5, in1=eb[:, :], op0=ALU.mult, op1=ALU.mult),
                  r=[pb, ebb], w=[qdecb])
                for c in range(4):
                    dcol = dprev[:, 0:1] if c == 0 else eb[:, c * 128 - 1:c * 128]
                    V(lambda e: e.tensor_scalar(out=q2[:, c * 128:(c + 1) * 128], in0=qdec[:, c * 128:(c + 1) * 128], scalar1=dcol, scalar2=None, op0=ALU.mult),
                      r=[qdecb, dprevb, ebb], pw=[q2b])
                p, pb = inproj_fm(hs, hsb, C_GK, 128)
                V(lambda e: e.tensor_tensor(out=kinv[:, :], in0=p[:, :], in1=enb[:, :], op=ALU.mult), r=[pb, enbb], w=[kinvb])
            if 'mqk' in STAGES:
                for kind in ("k", "q"):
                    for h in range(2):
                        col = (C_MKA, C_MKB)[h] if kind == "k" else (C_MQA, C_MQB)[h]
                        gcol = 2 if kind == "k" else 1
                        p, pb = inproj_fm(hs, hsb, col, 64)
                        LV = MQK_LEVEL[0]
                        if LV >= 2: A(lambda e: e.activation(out=sq[:, :], in_=p[0:64, :], func=AF.Square), r=[pb], w=[sqb])
                        pss, pssb = nextps()
                        if LV >= 3: MM(lambda e: e.matmul(pss[0:64, :], lhsT=ones_b[0:64, 0:64], rhs=sq[:, :], start=True, stop=True), r=[onb, sqb], w=[pssb])
                        if LV >= 4: A(lambda e: e.activation(out=rs[:, :], in_=pss[0:64, :], func=AF.Ln, scale=1.0 / 64, bias=epsc[0:64, 0:1]), r=[pssb, epsb], w=[rsb])
                        if LV >= 4: A(lambda e: e.activation(out=rs[:, :], in_=rs[:, :], func=AF.Exp, scale=-0.5), r=[rsb], w=[rsb])
                        if LV >= 5: A(lambda e: e.activation(out=kg[:, :], in_=p[0:64, :], func=AF.Identity, scale=pp_sb[0:64, gcol:gcol + 1]),
                          r=[pb, ppb], w=[kgb])
                        pr, prb = nextps()
                        if LV >= 6: MM(lambda e: e.matmul(pr[0:64, :], lhsT=RT, rhs=kg[:, :], start=True, stop=True), r=[cbb, kgb], w=[prb])
                        if LV >= 7: V(lambda e: e.tensor_tensor(out=ta[:, :], in0=kg[:, :], in1=cs_sb[:, 0, :], op=ALU.mult), r=[kgb, csb], w=[tab])
                        if LV >= 8: V(lambda e: e.tensor_tensor(out=tb[:, :], in0=pr[0:64, :], in1=cs_sb[:, 1, :], op=ALU.mult), r=[prb, csb], w=[tbb])
                        if LV >= 9: V(lambda e: e.tensor_tensor(out=ta[:, :], in0=ta[:, :], in1=tb[:, :], op=ALU.add), r=[tab, tbb], w=[tab])
                        if kind == "k":
                            if LV >= 10: V(lambda e: e.tensor_tensor(out=kaug[h][0:64, ts:ts + 512], in0=ta[:, :], in1=rs[:, :], op=ALU.mult),
                              r=[tab, rsb], pw=[kaugb[h]])
                            if LV >= 11: V(lambda e: e.tensor_reduce(out=km[:, :], in_=kaug[h][0:64, ts:ts + 512].rearrange("p (b k) -> p b k", k=256), axis=AX.X, op=ALU.add),
                              r=[kaugb[h]], w=[kmb])
                            if LV >= 11: V(lambda e: e.tensor_scalar(out=KmT[:, h, 2 * t:2 * t + 2], in0=km[:, :], scalar1=1.0 / 256, scalar2=None, op0=ALU.mult),
                              r=[kmb], pw=[KmTb])
                        else:
                            if LV >= 10: V(lambda e: e.scalar_tensor_tensor(out=qaug[h][0:64, :], in0=ta[:, :], scalar=0.125, in1=rs[:, :], op0=ALU.mult, op1=ALU.mult),
                              r=[tab, rsb], pw=[qaugb[h]])
                            for s in range(4 if ('gate' in STAGES and LV >= 12) else 0):
                                ob = (ts + s * 128) // 256
                                pgt, pgtb = nextps()
                                MM(lambda e: e.matmul(pgt[:, 0:32], lhsT=qaug[h][0:64, s * 128:(s + 1) * 128], rhs=KmT[:, h, :], start=True, stop=True),
                                   r=[qaugb[h], KmTb], w=[pgtb])
                                V(lambda e: e.memset(gsb[:, :], -1e30), w=[gsbb])
                                if ob > 0:
                                    V(lambda e: e.tensor_copy(out=gsb[:, 0:ob], in_=pgt[:, 0:ob]), r=[pgtb], w=[gsbb])
                                V(lambda e: e.max(out=mx8[:, :], in_=gsb[:, :]), r=[gsbb], w=[mx8b])
                                V(lambda e: e.tensor_tensor(out=gsb[:, :], in0=gsb[:, :], in1=mx8[:, 2:3].to_broadcast([128, 32]), op=ALU.is_lt),
                                  r=[gsbb, mx8b], w=[gsbb])
                                V(lambda e: e.tensor_scalar(out=mb96[:, 64:96], in0=gsb[:, :], scalar1=NEG, scalar2=None, op0=ALU.mult),
                                  r=[gsbb], w=[mb96b])
                                V(lambda e: e.memset(mb96[:, 64 + ob:65 + ob], 0.0), w=[mb96b])
                                MM(lambda e: e.transpose(out=pst[0:96, 0:128], in_=mb96[:, :], identity=ident_b), r=[mb96b, cbb], w=[pstb])
                                A(lambda e: e.copy(out=qaug[h][64:96, s * 128:(s + 1) * 128], in_=pst[64:96, 0:128]), r=[pstb], pw=[qaugb[h]])
            if 'mlc' in STAGES:
                for i, (col, dst, dstb) in enumerate(((C_LQ, qc, qcb), (C_LK, kc, kcb))):
                    p, pb = inproj_fm(hs, hsb, col, 64)
                    A(lambda e: e.copy(out=xq[i][:, 3:515], in_=p[0:64, :]), r=[pb], w=[xqb[i]])
                    c0 = 3 + 4 * i
                    A(lambda e: e.activation(out=cva[:, :], in_=xq[i][:, 0:512], func=AF.Identity, scale=ppc[0:64, c0:c0 + 1]), r=[xqb[i], ppcb], w=[cvab])
                    for wi in (1, 2, 3):
                        A(lambda e: e.activation(out=ta[:, :], in_=xq[i][:, wi:wi + 512], func=AF.Identity, scale=ppc[0:64, c0 + wi:c0 + wi + 1]), r=[xqb[i], ppcb], w=[tab])
                        if wi < 3:
                            V(lambda e: e.tensor_tensor(out=cva[:, :], in0=cva[:, :], in1=ta[:, :], op=ALU.add), r=[cvab, tab], w=[cvab])
                        else:
                            V(lambda e: e.tensor_tensor(out=dst[:, :], in0=cva[:, :], in1=ta[:, :], op=ALU.add), r=[cvab, tab], w=[dstb])
                    V(lambda e: e.tensor_copy(out=xq[i][:, 0:3], in_=xq[i][:, 512:515]), r=[xqb[i]], w=[xqb[i]])
            if 'tm' in STAGES:
                for s in range(4):
                    kt = t * 4 + s
                    for (pbank, pbk, c0, n) in ((PS[2], PSb[2], C_TM1, 384), (PS[3], PSb[3], C_TM2, 258)):
                        for c in range(8):
                            MM(lambda e: e.matmul(pbank[:, 0:n], lhsT=hs[:, c, s * 128:(s + 1) * 128], rhs=w_sb[:, c, c0:c0 + n], start=(c == 0), stop=(c == 7)),
                               r=[wb, hsb], w=[pbk], inc=(c == 7))
                    p2_, p3_ = PS[2], PS[3]
                    if TM_LEVEL[0] >= 2: A(lambda e: e.copy(out=vtm[:, s, :], in_=p2_[:, 0:384]), r=[PSb[2]], pw=[vtmb])
                    if TM_LEVEL[0] >= 3: V(lambda e: e.tensor_copy(out=Vst[:, kt, :, 0:64], in_=vtm[:, s, 128:256].rearrange("p (h d) -> p h d", d=64)), r=[vtmb], pw=[Vstb])
                    if TM_LEVEL[0] >= 4: V(lambda e: e.tensor_copy(out=vml[:, s, 0:128], in_=vtm[:, s, 256:384]), r=[vtmb], pw=[vmlb])
                    if TM_LEVEL[0] >= 5: A(lambda e: e.activation(out=ee[:, :], in_=p3_[:, 0:256], func=AF.Exp, scale=-1.0), r=[PSb[3]], w=[eeb])
                    if TM_LEVEL[0] >= 5: V(lambda e: e.tensor_scalar(out=ee[:, :], in0=ee[:, :], scalar1=1.0, scalar2=None, op0=ALU.add), r=[eeb], w=[eeb])
                    if TM_LEVEL[0] >= 5: V(lambda e: e.reciprocal(out=ee[:, :], in_=ee[:, :]), r=[eeb], w=[eeb])
                    if TM_LEVEL[0] >= 6: V(lambda e: e.tensor_tensor(out=gmo[:, s, :], in0=ee[:, 0:128], in1=bc_sb[:, 128:256], op=ALU.mult), r=[eeb, bcb], pw=[gmob])
                    if TM_LEVEL[0] >= 7: V(lambda e: e.tensor_tensor(out=gmg[:, s, :], in0=ee[:, 128:256], in1=p3_[:, 128:256], op=ALU.mult), r=[eeb, PSb[3]], pw=[gmgb])
                    if TM_LEVEL[0] >= 8: G(lambda e: e.tensor_tensor(out=gmg[:, s, :], in0=gmg[:, s, :], in1=bc_sb[:, 0:128], op=ALU.mult), r=[gmgb, bcb], pw=[gmgb])
                    if TM_LEVEL[0] >= 9: V(lambda e: e.tensor_tensor(out=li[:, s:s + 1], in0=p3_[:, 256:257], in1=bc_sb[:, 256:257], op=ALU.add), r=[PSb[3], bcb], pw=[lib])
                    if TM_LEVEL[0] >= 10: A(lambda e: e.activation(out=nlf[:, s:s + 1], in_=p3_[:, 257:258], func=AF.Exp, scale=-1.0, bias=neg_sb[:, 1:2]), r=[PSb[3], negb_], pw=[nlfb])
                    if TM_LEVEL[0] >= 10: A(lambda e: e.activation(out=nlf[:, s:s + 1], in_=nlf[:, s:s + 1], func=AF.Ln, bias=1.0), r=[nlfb], pw=[nlfb])
            def chainA():
                if 'gla' in STAGES:
                    for c in range(4):
                        cs_ = slice(c * 128, (c + 1) * 128)
                        n = t * 4 + c
                        Wold, Woldb = Wst[n % 2], Wstb[n % 2]
                        Wnew, Wnewb = Wst[(n + 1) % 2], Wstb[(n + 1) % 2]
                        dcol = dprev[:, 0:1] if c == 0 else eb[:, c * 128 - 1:c * 128]
                        if GL[0] >= 1: MM(lambda e: e.transpose(out=pst[:, 128:256], in_=kinv[:, cs_], identity=ident_b), r=[kinvb, cbb], w=[pstb])
                        yield
                        if GL[0] >= 2: A(lambda e: e.copy(out=kinvT[:, :], in_=pst[:, 128:256]), r=[pstb], w=[kinvTb])
                        yield
                        pa, pab = PS[4], PSb[4]
                        if GL[0] >= 3: MM(lambda e: e.matmul(pa[:, 0:128], lhsT=kinv[:, cs_], rhs=qdec[:, cs_], start=True, stop=True), r=[kinvb, qdecb], w=[pab])
                        yield
                        if GL[0] >= 4: V(lambda e: e.tensor_tensor(out=attn[:, :], in0=pa[:, 0:128], in1=mask_f, op=ALU.mult), r=[pab, cfb], w=[attnb])
                        yield
                        po, pob = PS[5], PSb[5]
                        if GL[0] >= 5: MM(lambda e: e.matmul(po[:, 0:128], lhsT=attn[:, :], rhs=vtm[:, c, 0:128], start=True, stop=False), r=[attnb, vtmb], w=[pob], inc=False)
                        yield
                        if GL[0] >= 6: MM(lambda e: e.matmul(po[:, 0:128], lhsT=q2[:, cs_], rhs=Wold[:, :], start=False, stop=True), r=[q2b, Woldb], w=[pob])
                        yield
                        if GL[0] >= 7: MM(lambda e: e.matmul(po[:, 128:256], lhsT=kinvT[:, :], rhs=vtm[:, c, 0:128], start=True, stop=True), r=[kinvTb, vtmb], w=[pob])
                        yield
                        if GL[0] >= 8: A(lambda e: e.activation(out=Wtmp[:, :], in_=Wold[:, :], func=AF.Identity, scale=dcol), r=[Woldb, dprevb, ebb], w=[Wtmpb])
                        yield
                        if GL[0] >= 9: V(lambda e: e.tensor_tensor(out=Wnew[:, :], in0=Wtmp[:, :], in1=po[:, 128:256], op=ALU.add), r=[Wtmpb, pob], w=[Wnewb])
                        yield
                        if GL[0] >= 10: A(lambda e: e.copy(out=obg[:, c, :], in_=po[:, 0:128]), r=[pob], pw=[obgb])
                        yield
                        if GL[0] >= 11: V(lambda e: e.tensor_tensor(out=ubg[:, c, :], in0=po[:, 0:128], in1=gmg[:, c, :], op=ALU.mult), r=[pob, gmgb], pw=[ubgb])
                        yield
                    if GL[0] >= 15: V(lambda e: e.tensor_copy(out=dprev[:, :], in_=eb[:, 511:512]), r=[ebb], w=[dprevb])
                    yield
                yield
            def chainA2():
                if 'ml' in STAGES:
                    for c in range(4):
                        cs_ = slice(c * 128, (c + 1) * 128)
                        A(lambda e: e.activation(out=Am[:, :], in_=SL_f, func=AF.Identity, scale=nlf[:, c:c + 1]), r=[cfb, nlfb], w=[Amb])
                        yield
                        A(lambda e: e.activation(out=nbc[:, :], in_=ones_f[:, :], func=AF.Identity, scale=nlf[:, c:c + 1]), r=[onfb, nlfb], w=[nbcb])
                        yield
                        pg, pgb = PS[6], PSb[6]
                        MM(lambda e: e.matmul(pg[:, 0:128], lhsT=Am[:, :], rhs=mask_f, start=True, stop=True), r=[Amb, cfb], w=[pgb])
                        yield
                        MM(lambda e: e.matmul(pg[:, 128:129], lhsT=mask_f, rhs=nlf[:, c:c + 1], start=True, stop=True), r=[cfb, nlfb], w=[pgb])
                        yield
                        MM(lambda e: e.matmul(pg[0:64, 129:130], lhsT=nbc[:, :], rhs=ones_f[:, 0:1], start=True, stop=True), r=[nbcb, onfb], w=[pgb])
                        yield
                        A(lambda e: e.activation(out=DT[:, :], in_=pg[:, 0:128], func=AF.Exp, scale=-1.0, bias=li[:, c:c + 1]), r=[pgb, lib], w=[DTb])
                        yield
                        A(lambda e: e.activation(out=st1[:, 1:2], in_=pg[:, 128:129], func=AF.Exp, scale=-1.0), r=[pgb], pw=[st1b])
                        yield
                        A(lambda e: e.activation(out=efl[:, :], in_=pg[0:64, 129:130], func=AF.Exp, scale=-1.0), r=[pgb], w=[eflb])
                        yield
                        G(lambda e: e.tensor_tensor(out=DTm[:, :], in0=DT[:, :], in1=mask_f, op=ALU.mult), r=[DTb, cfb], w=[DTmb])
                        yield
                        psx, psxb = PS[6], PSb[6]
                        MM(lambda e: e.matmul(psx[:, 0:128], lhsT=kc[:, cs_], rhs=qc[:, cs_], start=True, stop=True), r=[kcb, qcb], w=[psxb])
                        yield
                        V(lambda e: e.scalar_tensor_tensor(out=sT[:, :], in0=psx[:, 0:128], scalar=1.0, in1=DTm[:, :], op0=ALU.mult, op1=ALU.mult),
                          r=[psxb, DTmb], w=[sTb])
                        yield
                        pn, pnb = PS[6], PSb[6]
                        MM(lambda e: e.matmul(pn[:, 0:129], lhsT=sT[:, :], rhs=vml[:, c, :], start=True, stop=True), r=[sTb, vmlb], w=[pnb])
                        yield
                        MM(lambda e: e.matmul(pn[:, 256:385], lhsT=qc[:, cs_], rhs=Cbf[:, :], start=True, stop=True), r=[qcb, Cbfb], w=[pnb])
                        yield
                        A(lambda e: e.activation(out=tot[:, :], in_=pn[:, 256:385], func=AF.Identity, scale=st1[:, 1:2]), r=[pnb, st1b], w=[totb])
                        yield
                        V(lambda e: e.tensor_tensor(out=tot[:, :], in0=tot[:, :], in1=pn[:, 0:129], op=ALU.add), r=[totb, pnb], w=[totb])
                        yield
                        A(lambda e: e.activation(out=st1[:, 2:3], in_=tot[:, 128:129], func=AF.Abs), r=[totb], pw=[st1b])
                        yield
                        V(lambda e: e.tensor_scalar(out=st1[:, 2:3], in0=st1[:, 2:3], scalar1=1.0, scalar2=None, op0=ALU.max), r=[st1b], pw=[st1b])
                        yield
                        V(lambda e: e.reciprocal(out=st1[:, 2:3], in_=st1[:, 2:3]), r=[st1b], pw=[st1b])
                        yield
                        A(lambda e: e.activation(out=hh[:, :], in_=tot[:, 0:128], func=AF.Identity, scale=st1[:, 2:3]), r=[totb, st1b], w=[hhb])
                        yield
                        A(lambda e: e.copy(out=obm[:, c, :], in_=hh[:, :]), r=[hhb], pw=[obmb])
                        yield
                        V(lambda e: e.tensor_tensor(out=ubm[:, c, :], in0=hh[:, :], in1=gmo[:, c, :], op=ALU.mult), r=[hhb, gmob], pw=[ubmb])
                        yield
                        MM(lambda e: e.transpose(out=pst[:, 256:320], in_=kc[:, cs_], identity=ident_b[0:64, 0:64]), r=[kcb, cbb], w=[pstb])
                        yield
                        A(lambda e: e.activation(out=kw[:, :], in_=pst[:, 256:320], func=AF.Identity, scale=DT[:, 127:128]), r=[pstb, DTb], w=[kwb])
                        yield
                        pk, pkb = PS[6], PSb[6]
                        MM(lambda e: e.matmul(pk[0:64, 0:129], lhsT=kw[:, :], rhs=vml[:, c, :], start=True, stop=True), r=[kwb, vmlb], w=[pkb])
                        yield
                        A(lambda e: e.activation(out=C32[:, :], in_=C32[:, :], func=AF.Identity, scale=efl[:, 0:1]), r=[C32b, eflb], w=[C32b])
                        yield
                        V(lambda e: e.tensor_tensor(out=C32[:, :], in0=C32[:, :], in1=pk[0:64, 0:129], op=ALU.add), r=[C32b, pkb], w=[C32b])
                        yield
                        A(lambda e: e.copy(out=Cbf[:, :], in_=C32[:, :]), r=[C32b], w=[Cbfb])
                        yield

                yield
            def chainB():
                if 'moba' in STAGES:
                    for h in range(2):
                        acc, accb = PS[2 + h], PSb[2 + h]
                        nk = (t + 1) * 4
                        for kt in range(nk):
                            r_ = kt - t * 4
                            q0 = 0 if r_ < 0 else r_ * 128
                            psS, psSb = PS[kt % 2], PSb[kt % 2]
                            diag = r_ >= 0
                            MM(lambda e: e.matmul(psS[:, q0:512], lhsT=kaug[h][0:96, kt * 128:(kt + 1) * 128], rhs=qaug[h][0:96, q0:512], start=True, stop=not diag),
                               r=[kaugb[h], qaugb[h]], w=[psSb], inc=not diag)
                            if diag:
                                MM(lambda e: e.matmul(psS[:, q0:512], lhsT=ident_b, rhs=CBd[:, 0:512 - q0], start=False, stop=True), r=[cbb], w=[psSb])
                            P_, P_b = Pb[kt % 2], Pbb[kt % 2]
                            A(lambda e: e.activation(out=P_[:, q0:512], in_=psS[:, q0:512], func=AF.Exp), r=[psSb], w=[P_b])
                            for s in range(max(r_, 0), 4):
                                MM(lambda e: e.matmul(acc[:, s * 65:(s + 1) * 65], lhsT=P_[:, s * 128:(s + 1) * 128], rhs=Vst[:, kt, h, :],
                                                      start=(kt == 0 and s == 0), stop=(kt == nk - 1), skip_group_check=True),
                                   r=[P_b, Vstb], w=[accb], inc=(s == 3))
                            yield
                        A(lambda e: e.copy(out=accs[:, :], in_=acc[:, 0:260]), r=[accb], w=[accsb])
                        accv = accs[:, 0:260].rearrange("p (s d) -> p s d", d=65)
                        V(lambda e: e.reciprocal(out=rec4[:, :], in_=accv[:, :, 64]), r=[accsb], w=[rec4b])
                        for s in range(4):
                            A(lambda e: e.activation(out=ytok[:, s, 128 + h * 64:192 + h * 64], in_=accs[:, s * 65:s * 65 + 64], func=AF.Identity, scale=rec4[:, s:s + 1]),
                              r=[accsb, rec4b], pw=[ytokb])

                yield
            gens = [chainA(), chainA2(), chainB()]
            while gens:
                for g_ in list(gens):
                    try:
                        next(g_)
                    except StopIteration:
                        gens.remove(g_)
            if 'out' in STAGES:
                for s in range(4):
                    scol = slice(s * 128, (s + 1) * 128)
                    srcs = ((obg[:, s, :], obgb, 512), (ubg[:, s, :], ubgb, 640), (ytok[:, s, 128:256], ytokb, 768), (obm[:, s, :], obmb, 896), (ubm[:, s, :], ubmb, 384))
                    for k_, (src, srcb, off) in enumerate(srcs):
                        MM(lambda e: e.transpose(out=pst[:, off:off + 128], in_=src, identity=ident_b), r=[srcb, cbb], w=[pstb], inc=(k_ == 4))
                    for br, ooff, uoff in ((0, 512, 640), (2, 896, 384)):
                        A(lambda e: e.activation(out=osq[:, :], in_=pst[:, ooff:ooff + 128], func=AF.Square), r=[pstb], w=[osqb])
                        pss, pssb = nextps()
                        MM(lambda e: e.matmul(pss[:, 0:128], lhsT=ones_b[:, :], rhs=osq[:, :], start=True, stop=True), r=[onb, osqb], w=[pssb])
                        A(lambda e: e.activation(out=rsn[:, :], in_=pss[:, 0:128], func=AF.Ln, scale=1.0 / 128, bias=epsc[:, 0:1]), r=[pssb, epsb], w=[rsnb])
                        A(lambda e: e.activation(out=rsn[:, :], in_=rsn[:, :], func=AF.Exp, scale=-0.5), r=[rsnb], w=[rsnb])
                        V(lambda e: e.tensor_tensor(out=yT_sb[:, br, scol], in0=pst[:, uoff:uoff + 128], in1=rsn[:, :], op=ALU.mult), r=[pstb, rsnb], pw=[yTb])
                    A(lambda e: e.copy(out=yT_sb[:, 1, scol], in_=pst[:, 768:896]), r=[pstb], pw=[yTb])
                S.dma("sync", yTr[:, :, ts:ts + 512], yT_sb[:, :, :], reads=[yTb], pwrites=[yTdb], sem_owner=yTb)
        S.finish([yTdb])
    return nc


NEG = -30000.0
GLA_K = 512; MOBA_W = 512; MQK = 256; MV = 512
OFF = {}
_splits = [("g_q",512),("g_k",512),("g_v",512),("g_g",512),("g_a",16),("m_q",512),("m_k",512),("m_v",512),
           ("l_q",256),("l_k",256),("l_v",512),("l_o",512),("l_i",4),("l_f",4),("br",3072)]
_o = 0
for n, s in _splits:
    OFF[n] = _o; _o += s

def p2_cols(g):
    r = lambda name, a, b: list(range(OFF[name] + a, OFF[name] + b))
    cols = []
    cols += r("g_q", g*128, g*128+128) + r("g_k", g*128, g*128+128) + r("g_a", 0, 16)
    cols += r("m_q", g*128, g*128+64) + r("m_q", g*128+64, g*128+128) + r("m_k", g*128, g*128+64) + r("m_k", g*128+64, g*128+128)
    cols += r("l_q", g*64, g*64+64) + r("l_k", g*64, g*64+64)
    cols += r("g_v", g*128, g*128+128) + r("m_v", g*128, g*128+128) + r("l_v", g*128, g*128+128)
    cols += r("l_o", g*128, g*128+128) + r("g_g", g*128, g*128+128) + r("l_i", g, g+1) + r("l_f", g, g+1)
    assert len(cols) == 1298
    return np.array(cols)

def consts(T):
    c = np.zeros((128, 1472), np.float32)
    j = np.arange(128)[:, None]; i = np.arange(128)[None, :]
    c[:, 0:128] = (j <= i)
    c[:, 128:256] = (j > i)
    c[:, 256:384] = np.eye(128)
    rs = np.ones((128, 512), np.float32); rs[:, [0, 128, 256, 384]] = 0
    c[:, 384:896] = rs
    ii = np.arange(512)[None, :]
    c[:, 896:1408] = np.where(j <= ii, 0.0, NEG)
    RT = np.zeros((64, 64), np.float32)
    for m in range(32):
        RT[m + 32, m] = -1.0
    for m in range(32, 64):
        RT[m - 32, m] = 1.0
    c[0:64, 1408:1472] = RT
    inv = (1.0 / (np.float32(10000.0) ** (np.arange(0, 64, 2, dtype=np.float32) / np.float32(64)))).astype(np.float32)
    ang = np.arange(T, dtype=np.float32)[:, None] * inv[None, :]
    cos = np.cos(ang).astype(np.float32).T; sin = np.sin(ang).astype(np.float32).T
    cosT = np.ascontiguousarray(np.concatenate([cos, cos], 0)); sinT = np.ascontiguousarray(np.concatenate([sin, sin], 0))
    onehot = (np.arange(T)[None, :] // 256 == np.arange(32)[:, None]).astype(np.float32)
    return dict(cst=c, cosT=cosT, sinT=sinT, onehot=onehot)

def prep_p2(I, l, g):
    w_in = I["w_in"][l]
    wsel = w_in[:, p2_cols(g)]
    w = np.ascontiguousarray(wsel.reshape(8, 128, 1298).transpose(1, 0, 2))
    pp = np.zeros((128, 16), np.float32)
    pp[:, 0] = I["gla_a_b"][l][g*128:(g+1)*128]
    pp[0:64, 1] = I["moba_qn_g"][l]; pp[0:64, 2] = I["moba_kn_g"][l]
    cw = I["mlstm_conv_w"][l]
    pp[0:64, 3:7] = cw[:, g*64:(g+1)*64].T
    pp[0:64, 7:11] = cw[:, 256 + g*64:256 + (g+1)*64].T
    bc = np.zeros((128, 258), np.float32)
    bc[:, 0:128] = I["gla_norm_g"][l][None, :]; bc[:, 128:256] = I["mlstm_norm_g"][l][None, :]
    bc[:, 256] = I["mlstm_i_b"][l][g]; bc[:, 257] = I["mlstm_f_b"][l][g]
    up = np.ascontiguousarray(I["gla_a_up"][l][:, g*128:(g+1)*128])
    return dict(w=w, pp=pp, bc=bc, up=up)


from concourse.bass_utils import run_bass_kernel_spmd
import ml_dtypes

_PROG = {}


def _prog(key, fn):
    if key not in _PROG:
        _PROG[key] = fn()
    return _PROG[key]


def _tiles(w, kc):
    K, N = w.shape
    return np.ascontiguousarray(w.reshape(kc, 128, N // 128, 128).transpose(2, 1, 0, 3))


def _pp(v):
    return np.ascontiguousarray(v.reshape(-1, 128).T)


def kernel(**inputs):
    I = {k: np.asarray(v) for k, v in inputs.items()}
    x = I["x"].astype(np.float32)
    B, T, Dm = x.shape
    L = I["w_in"].shape[0]
    NTOK = T // 4
    cores = list(range(8))
    xT = [np.ascontiguousarray(x[c // 4, (c % 4) * NTOK:(c % 4 + 1) * NTOK, :].T) for c in cores]
    nc0 = _prog("p1", lambda: build_p3(NTOK, False, first_only=True))
    res = run_bass_kernel_spmd(nc0, [dict(xT=xT[c], g1n=_pp(I["norm1_g"][0])) for c in cores], core_ids=cores).results
    hn = [np.asarray(res[c]["hno"]) for c in cores]
    cst = consts(T)
    nc2 = _prog("p2", lambda: build_p2(T))
    for l in range(L):
        last = (l == L - 1)
        in2 = []
        for c in cores:
            b, g = c // 4, c % 4
            d = prep_p2(I, l, g)
            d.update(cst)
            d["hnT"] = np.ascontiguousarray(np.concatenate([hn[b * 4 + j] for j in range(4)], axis=1))
            in2.append(d)
        res2 = run_bass_kernel_spmd(nc2, in2, core_ids=cores).results
        yT = [np.asarray(res2[c]["yT"]) for c in cores]
        wl = dict(wg=_tiles(np.ascontiguousarray(I["w_in"][l][:, OFF["br"]:]), 8), gbias=_pp(I["gate_b"][l]),
                  wbr=np.concatenate([_tiles(I["w_br_gla"][l], 4), _tiles(I["w_br_moba"][l], 4), _tiles(I["w_br_mlstm"][l], 4)], 0),
                  wo=_tiles(I["w_out"][l], 8), g2=_pp(I["norm2_g"][l]), w1=_tiles(I["w_ff1"][l], 8), w2=_tiles(I["w_ff2"][l], 32),
                  g1n=_pp(I["norm1_g"][min(l + 1, L - 1)]))
        nc3 = _prog("p3_%d" % last, lambda: build_p3(NTOK, last))
        in3 = []
        for c in cores:
            b, j = c // 4, c % 4
            d = dict(wl)
            d["xT"] = xT[c]
            d["hnT"] = hn[c]
            d["yT"] = np.ascontiguousarray(np.concatenate([yT[b * 4 + g][:, j * NTOK:(j + 1) * NTOK] for g in range(4)], axis=0))
            in3.append(d)
        res3 = run_bass_kernel_spmd(nc3, in3, core_ids=cores).results
        xT = [np.asarray(res3[c]["xo"]) for c in cores]
        if not last:
            hn = [np.asarray(res3[c]["hno"]) for c in cores]
    out = np.empty((B, T, Dm), np.float32)
    for c in cores:
        out[c // 4, (c % 4) * NTOK:(c % 4 + 1) * NTOK, :] = xT[c].T
    return out
```
